# Optimizing a Trainium2 kernel written in Bass

```python
import math
import jax, jax.numpy as jnp
from jax import lax
import numpy as np

D_MODEL = 1024
BATCH = 8
SEQ = 2048
DEPTH = 4

BRANCH_WIDTH = D_MODEL // 2
N_BRANCH = 3
N_IN_CHUNKS = 6
SSM_GROUP = 16
SSM_GROUPS = BRANCH_WIDTH // SSM_GROUP
SSM_STATE = 64
DT_MIN = 1e-3
DT_MAX = 1e-1
CONF_KERNEL = 31
SCONV_KERNEL = 3
N_GROUPS = 4
EXPERTS_PER_GROUP = 8
N_EXPERTS = N_GROUPS * EXPERTS_PER_GROUP
TOP_K = 2
D_FF_EXPERT = D_MODEL // 2
MOE_BLOCK = 128
NORM_EPS = 1e-6

kernel_name = 'hybrid_s5_conformer_shortconv_hmoe_adaln'


def rms_norm(x, g):
    x32 = x.astype(jnp.float32)
    y = x32 * lax.rsqrt(jnp.mean(x32 * x32, axis=-1, keepdims=True) + NORM_EPS)
    return (y * g.astype(jnp.float32)).astype(x.dtype)


def layer_norm(x, g, b):
    x32 = x.astype(jnp.float32)
    xc = x32 - jnp.mean(x32, axis=-1, keepdims=True)
    y = xc * lax.rsqrt(jnp.mean(xc * xc, axis=-1, keepdims=True) + NORM_EPS)
    return (y * g.astype(jnp.float32) + b.astype(jnp.float32)).astype(x.dtype)


def causal_depthwise_conv(x, w):
    k, ch = w.shape
    xp = jnp.pad(x, ((0, 0), (k - 1, 0), (0, 0)))
    return lax.conv_general_dilated(xp, w[:, None, :].astype(x.dtype), window_strides=(1,),
                                    padding='VALID', dimension_numbers=('NWC', 'WIO', 'NWC'),
                                    feature_group_count=ch)


def _complex_linear_combine(left, right):
    ar1, ai1, br1, bi1 = left
    ar2, ai2, br2, bi2 = right
    ar = ar1 * ar2 - ai1 * ai2
    ai = ar1 * ai2 + ai1 * ar2
    br = ar2 * br1 - ai2 * bi1 + br2
    bi = ar2 * bi1 + ai2 * br1 + bi2
    return (ar, ai, br, bi)


def s5_branch(u, lam_re, lam_im, log_step, b_re, b_im, c_re, c_im, d_skip, w_glu):
    f32 = jnp.float32
    bsz, seq, _ = u.shape
    uf = u.astype(f32).reshape(bsz, seq, SSM_GROUPS, SSM_GROUP)
    lr, li = lam_re.astype(f32), lam_im.astype(f32)
    step = jnp.exp(log_step.astype(f32))[:, None]
    mag = jnp.exp(lr * step)
    ar, ai = mag * jnp.cos(li * step), mag * jnp.sin(li * step)
    den = lr * lr + li * li
    nr, ni = ar - 1.0, ai
    kr = (nr * lr + ni * li) / den
    ki = (ni * lr - nr * li) / den
    br, bi = b_re.astype(f32), b_im.astype(f32)
    bbr = kr[..., None] * br - ki[..., None] * bi
    bbi = kr[..., None] * bi + ki[..., None] * br
    bu_r = jnp.einsum('gph,bsgh->bsgp', bbr, uf)
    bu_i = jnp.einsum('gph,bsgh->bsgp', bbi, uf)
    a_r = jnp.broadcast_to(ar[None, None], (1, seq) + ar.shape)
    a_i = jnp.broadcast_to(ai[None, None], (1, seq) + ai.shape)
    _, _, xr, xi = lax.associative_scan(_complex_linear_combine, (a_r, a_i, bu_r, bu_i), axis=1)
    y = (jnp.einsum('ghp,bsgp->bsgh', c_re.astype(f32), xr)
         - jnp.einsum('ghp,bsgp->bsgh', c_im.astype(f32), xi))
    y = y.reshape(bsz, seq, BRANCH_WIDTH) + d_skip.astype(f32) * uf.reshape(bsz, seq, BRANCH_WIDTH)
    y = jax.nn.gelu(y).astype(u.dtype)
    return y * jax.nn.sigmoid(y @ w_glu)


def conformer_branch(v, g, dw_w, dw_b, ln_g, ln_b):
    z = v * jax.nn.sigmoid(g)
    z = causal_depthwise_conv(z, dw_w) + dw_b
    z = layer_norm(z, ln_g, ln_b)
    return jax.nn.silu(z)


def short_conv_branch(gate_b, gate_c, hv, sconv_w):
    return gate_b * causal_depthwise_conv(gate_c * hv, sconv_w)


def hybrid_mixer(h, w_in, lam_re, lam_im, log_step, ssm_b_re, ssm_b_im, ssm_c_re, ssm_c_im, ssm_d,
                 w_glu, conf_dw_w, conf_dw_b, conf_ln_g, conf_ln_b, sconv_w, w_branch, w_gate, b_gate,
                 w_out):
    bsz, seq, d = h.shape
    u_ssm, conf_v, conf_g, sc_b, sc_c, sc_h = jnp.split(h @ w_in, N_IN_CHUNKS, axis=-1)
    y_ssm = s5_branch(u_ssm, lam_re, lam_im, log_step, ssm_b_re, ssm_b_im, ssm_c_re, ssm_c_im,
                      ssm_d, w_glu)
    y_conf = conformer_branch(conf_v, conf_g, conf_dw_w, conf_dw_b, conf_ln_g, conf_ln_b)
    y_sc = short_conv_branch(sc_b, sc_c, sc_h, sconv_w)
    ys = jnp.stack([y_ssm, y_conf, y_sc], axis=2)
    branches = jnp.einsum('bsnw,nwd->bsnd', ys, w_branch)
    gates = jax.nn.sigmoid((h @ w_gate + b_gate).reshape(bsz, seq, N_BRANCH, d))
    return jnp.einsum('bsnd,bsnd->bsd', gates, branches) @ w_out


def grouped_experts(x, expert_idx, w1, w3, w2):
    t, d = x.shape
    n_assign = t * TOP_K
    flat_e = expert_idx.reshape(n_assign).astype(jnp.int32)
    flat_tok = jnp.arange(n_assign, dtype=jnp.int32) // TOP_K
    order = jnp.argsort(flat_e, stable=True)
    sorted_e = flat_e[order]
    counts = jnp.bincount(flat_e, length=N_EXPERTS).astype(jnp.int32)
    padded = (counts + MOE_BLOCK - 1) // MOE_BLOCK * MOE_BLOCK
    pad_end = jnp.cumsum(padded)
    pad_start = pad_end - padded
    start = jnp.cumsum(counts) - counts
    dest = pad_start[sorted_e] + (jnp.arange(n_assign, dtype=jnp.int32) - start[sorted_e])
    n_blocks = -(-n_assign // MOE_BLOCK) + N_EXPERTS
    cap = n_blocks * MOE_BLOCK
    slot_tok = jnp.full((cap,), t, jnp.int32).at[dest].set(flat_tok[order])
    block_e = jnp.minimum(jnp.searchsorted(pad_end, jnp.arange(n_blocks, dtype=jnp.int32) * MOE_BLOCK,
                                           side='right'), N_EXPERTS - 1)
    x_pad = jnp.concatenate([x, jnp.zeros((1, d), x.dtype)], axis=0)
    xb = x_pad[slot_tok].reshape(n_blocks, MOE_BLOCK, d)

    def run_block(args):
        xblk, e = args
        hid = jax.nn.silu(xblk @ w1[e]) * (xblk @ w3[e])
        return hid @ w2[e]

    yb = lax.map(run_block, (xb, block_e)).reshape(cap, d)
    slot_of_assign = jnp.zeros((n_assign,), jnp.int32).at[order].set(dest)
    return yb[slot_of_assign].reshape(t, TOP_K, d)


def hier_moe(h, w_rg, b_rg, w_re, b_re, w1, w3, w2):
    bsz, seq, d = h.shape
    t = bsz * seq
    x = h.reshape(t, d)
    tok = jnp.arange(t)
    grp_logits = (x @ w_rg + b_rg).astype(jnp.float32)
    grp_prob = jax.nn.softmax(grp_logits, axis=-1)
    g_sel = jnp.argmax(grp_logits, axis=-1)
    g_w = grp_prob[tok, g_sel][:, None]
    exp_logits = (x @ w_re + b_re).astype(jnp.float32).reshape(t, N_GROUPS, EXPERTS_PER_GROUP)
    top_v, top_i = lax.top_k(exp_logits[tok, g_sel], TOP_K)
    e_w = jax.nn.softmax(top_v, axis=-1) * g_w
    expert_idx = g_sel[:, None] * EXPERTS_PER_GROUP + top_i
    y = grouped_experts(x, expert_idx, w1, w3, w2)
    return jnp.einsum('tk,tkd->td', e_w.astype(x.dtype), y).reshape(bsz, seq, d)


def setup_inputs(seed: int = 0) -> dict:
    key = jax.random.key(seed)
    ks = jax.random.split(key, 40)
    nrm = jax.random.normal
    L, D, W = DEPTH, D_MODEL, BRANCH_WIDTH
    G, P, H = SSM_GROUPS, SSM_STATE, SSM_GROUP
    E, F = N_EXPERTS, D_FF_EXPERT
    f32 = jnp.float32
    inp = {}
    inp['x'] = nrm(ks[0], (BATCH, SEQ, D), f32)
    inp['c'] = nrm(ks[1], (BATCH, D), f32)
    inp['norm_mix_g'] = 1.0 + 0.02 * nrm(ks[2], (L, D), f32)
    inp['norm_ffn_g'] = 1.0 + 0.02 * nrm(ks[3], (L, D), f32)
    inp['w_ada'] = nrm(ks[4], (L, D, 6 * D), f32) * (0.5 * D ** -0.5)
    inp['b_ada'] = 0.01 * nrm(ks[5], (L, 6 * D), f32)
    inp['w_in'] = nrm(ks[6], (L, D, N_IN_CHUNKS * W), f32) * D ** -0.5
    inp['lam_re'] = -0.5 + 0.01 * nrm(ks[7], (L, G, P), f32)
    inp['lam_im'] = (math.pi * jnp.arange(P, dtype=f32))[None, None, :] + 0.01 * nrm(ks[8], (L, G, P), f32)
    inp['log_step'] = jax.random.uniform(ks[9], (L, G), f32, minval=math.log(DT_MIN), maxval=math.log(DT_MAX))
    inp['ssm_b_re'] = nrm(ks[10], (L, G, P, H), f32) * (2 * H) ** -0.5
    inp['ssm_b_im'] = nrm(ks[11], (L, G, P, H), f32) * (2 * H) ** -0.5
    inp['ssm_c_re'] = nrm(ks[12], (L, G, H, P), f32) * (2 * P) ** -0.5
    inp['ssm_c_im'] = nrm(ks[13], (L, G, H, P), f32) * (2 * P) ** -0.5
    inp['ssm_d'] = nrm(ks[14], (L, W), f32)
    inp['w_glu'] = nrm(ks[15], (L, W, W), f32) * W ** -0.5
    inp['conf_dw_w'] = nrm(ks[16], (L, CONF_KERNEL, W), f32) * CONF_KERNEL ** -0.5
    inp['conf_dw_b'] = 0.01 * nrm(ks[17], (L, W), f32)
    inp['conf_ln_g'] = 1.0 + 0.02 * nrm(ks[18], (L, W), f32)
    inp['conf_ln_b'] = 0.01 * nrm(ks[19], (L, W), f32)
    inp['sconv_w'] = nrm(ks[20], (L, SCONV_KERNEL, W), f32) * SCONV_KERNEL ** -0.5
    inp['w_branch'] = nrm(ks[21], (L, N_BRANCH, W, D), f32) * W ** -0.5
    inp['w_gate'] = nrm(ks[22], (L, D, N_BRANCH * D), f32) * D ** -0.5
    inp['b_gate'] = 0.01 * nrm(ks[23], (L, N_BRANCH * D), f32)
    inp['w_out'] = nrm(ks[24], (L, D, D), f32) * D ** -0.5
    inp['w_router_group'] = nrm(ks[25], (L, D, N_GROUPS), f32) * D ** -0.5
    inp['b_router_group'] = 0.01 * nrm(ks[26], (L, N_GROUPS), f32)
    inp['w_router_expert'] = nrm(ks[27], (L, D, E), f32) * D ** -0.5
    inp['b_router_expert'] = 0.01 * nrm(ks[28], (L, E), f32)
    inp['w_exp_gate'] = nrm(ks[29], (L, E, D, F), f32) * D ** -0.5
    inp['w_exp_up'] = nrm(ks[30], (L, E, D, F), f32) * D ** -0.5
    inp['w_exp_down'] = nrm(ks[31], (L, E, F, D), f32) * F ** -0.5
    inp['final_norm_g'] = 1.0 + 0.02 * nrm(ks[32], (D,), f32)
    return inp


def reference(x, c, norm_mix_g, norm_ffn_g, w_ada, b_ada, w_in, lam_re, lam_im, log_step,
              ssm_b_re, ssm_b_im, ssm_c_re, ssm_c_im, ssm_d, w_glu, conf_dw_w, conf_dw_b,
              conf_ln_g, conf_ln_b, sconv_w, w_branch, w_gate, b_gate, w_out, w_router_group,
              b_router_group, w_router_expert, b_router_expert, w_exp_gate, w_exp_up, w_exp_down,
              final_norm_g):
    cond = jax.nn.silu(c)
    for l in range(DEPTH):
        mod = (cond @ w_ada[l] + b_ada[l])[:, None, :]
        sh1, sc1, g1, sh2, sc2, g2 = jnp.split(mod, 6, axis=-1)
        h = rms_norm(x, norm_mix_g[l]) * (1.0 + sc1) + sh1
        x = x + g1 * hybrid_mixer(h, w_in[l], lam_re[l], lam_im[l], log_step[l], ssm_b_re[l],
                                  ssm_b_im[l], ssm_c_re[l], ssm_c_im[l], ssm_d[l], w_glu[l],
                                  conf_dw_w[l], conf_dw_b[l], conf_ln_g[l], conf_ln_b[l],
                                  sconv_w[l], w_branch[l], w_gate[l], b_gate[l], w_out[l])
        h = rms_norm(x, norm_ffn_g[l]) * (1.0 + sc2) + sh2
        x = x + g2 * hier_moe(h, w_router_group[l], b_router_group[l], w_router_expert[l],
                              b_router_expert[l], w_exp_gate[l], w_exp_up[l], w_exp_down[l])
    return rms_norm(x, final_norm_g)
```

```python
import math
from contextlib import ExitStack
import numpy as np
import concourse.bass as bass
import concourse.mybir as mybir
from concourse.bass_utils import run_bass_kernel_spmd

F32 = mybir.dt.float32
BF16 = mybir.dt.bfloat16
I32 = mybir.dt.int32
ALU = mybir.AluOpType
AF = mybir.ActivationFunctionType
AX = mybir.AxisListType

D = 1024
S = 2048
W = 512
NL = 4
NT = 16
NE = 32
NBLK = 64
CAP = NBLK * 128
EPS = 1e-6
TWO_PI = 2.0 * math.pi

ENGS = ['pe', 'act', 'dve', 'pool', 'sp']
SAME_ENG_SYNC = True


class Prog:
    def __init__(self, nc, ndma=8):
        self.nc = nc
        self.ops = {e: [] for e in ENGS}
        self.cnt = {e: 0 for e in ENGS}
        self.seen = {e: {} for e in ENGS}
        self.res = {}
        self.ndma = ndma
        self.dma_i = {e: 0 for e in ENGS}
        self.dma_exp = {}
        self.semkeys = set()

    def _need(self, eng, dep, waits):
        if dep is None:
            return
        key, val = dep
        if key == ('E', eng) and (eng == 'pe' or not SAME_ENG_SYNC):
            return
        if self.seen[eng].get(key, 0) >= val:
            return
        if waits.get(key, 0) < val:
            waits[key] = val

    def _deps(self, eng, reads, writes, waits):
        for r in reads:
            st = self.res.get(r)
            if st:
                self._need(eng, st[0], waits)
        for w in writes:
            st = self.res.get(w)
            if st:
                self._need(eng, st[0], waits)
                for d in st[1]:
                    self._need(eng, d, waits)

    def _commit(self, eng, mydep, reads, writes, waits):
        for key, val in waits.items():
            self.seen[eng][key] = val
        for r in reads:
            self.res.setdefault(r, [None, []])[1].append(mydep)
        for w in writes:
            self.res[w] = [mydep, []]

    def op(self, eng, fn, reads=(), writes=()):
        waits = {}
        self._deps(eng, reads, writes, waits)
        self.cnt[eng] += 1
        key = ('E', eng)
        self.semkeys.add(key)
        mydep = (key, self.cnt[eng])
        self.ops[eng].append((list(waits.items()), fn, key, 1))
        self._commit(eng, mydep, reads, writes, waits)
        return mydep

    def dma(self, q, fn, reads=(), writes=()):
        i = self.dma_i[q] % self.ndma
        self.dma_i[q] += 1
        skey = ('D', q, i)
        self.semkeys.add(skey)
        prev = self.dma_exp.get(skey, 0)
        waits = {}
        if prev > 0:
            self._need(q, (skey, prev), waits)
        self._deps(q, reads, writes, waits)
        self.dma_exp[skey] = prev + 16
        mydep = (skey, prev + 16)
        self.ops[q].append((list(waits.items()), fn, skey, 16))
        self._commit(q, mydep, reads, writes, waits)
        return mydep

    def barrier(self):
        deps = [(('E', e), self.cnt[e]) for e in ENGS if self.cnt[e] > 0]
        deps += [(k, v) for k, v in self.dma_exp.items()]
        for e in ENGS:
            waits = {}
            for d in deps:
                self._need(e, d, waits)
            if waits:
                self.ops[e].append((list(waits.items()), None, None, 0))
                for key, val in waits.items():
                    self.seen[e][key] = val
        self.res = {}

    def emit(self, stack):
        nc = self.nc
        sems = {}
        for k in sorted(self.semkeys, key=str):
            sems[k] = stack.enter_context(nc.semaphore("s_" + "_".join(str(x) for x in k)))
        block = stack.enter_context(nc.Block())
        battr = {'pe': 'tensor', 'act': 'scalar', 'dve': 'vector', 'pool': 'gpsimd', 'sp': 'sync'}

        def mk(e):
            def body(eng):
                for waits, fn, key, inc in self.ops[e]:
                    for wk, wv in waits:
                        eng.wait_ge(sems[wk], wv)
                    if fn is not None:
                        ins = fn(eng)
                        ins.then_inc(sems[key], inc)
            return body
        for e in ENGS:
            if self.ops[e]:
                getattr(block, battr[e])(mk(e))


def _I(name, *args, **kw):
    return lambda e: getattr(e, name)(*args, **kw)


class Arena:
    def __init__(self, nc, start=16576, limit=229344 - 64):
        self.nc, self.off, self.limit, self.n = nc, start, limit, 0

    def alloc(self, name, shape, dt):
        sz = {F32: 4, BF16: 2, I32: 4}[dt]
        nb = sz
        for s in shape[1:]:
            nb *= s
        nb = (nb + 31) // 32 * 32
        self.n += 1
        t = self.nc.alloc_sbuf_tensor_at("%s_%d" % (name, self.n), list(shape), dt, offset=self.off)
        self.off += nb
        self.peak = max(getattr(self, 'peak', 0), self.off)
        assert self.off <= self.limit, ("SBUF overflow", name, self.off)
        return t

    def mark(self):
        return self.off

    def release(self, m):
        self.off = m


class _Stop(Exception):
    pass


def build_program(n_layers=NL, taps=(), do_final=True):
    nc = bass.Bass("TRN2", target_bir_lowering=False)
    try:
        return _build_body(nc, n_layers, taps, do_final)
    except _Stop as e:
        return e.args[0]


def _build_body(nc, n_layers, taps, do_final):

    def din(name, shape, dt=F32):
        return nc.dram_tensor(name, list(shape), dt, kind="ExternalInput").ap()
    x_in = din("x", [S, D])
    c_in = din("c", [D])
    norm_mix_g = din("norm_mix_g", [NL, D])
    norm_ffn_g = din("norm_ffn_g", [NL, D])
    w_ada = din("w_ada", [NL, D, 6 * D])
    b_ada = din("b_ada", [NL, 6 * D])
    w_in = din("w_in", [NL, D, 6 * W])
    lam_re = din("lam_re", [NL, 32, 64])
    lam_im = din("lam_im", [NL, 32, 64])
    log_step = din("log_step", [NL, 32])
    ssm_b_re = din("ssm_b_re", [NL, 32, 64, 16])
    ssm_b_im = din("ssm_b_im", [NL, 32, 64, 16])
    ssm_c_re = din("ssm_c_re", [NL, 32, 16, 64])
    ssm_c_im = din("ssm_c_im", [NL, 32, 16, 64])
    ssm_d = din("ssm_d", [NL, W])
    w_glu = din("w_glu", [NL, W, W])
    conf_dw_w = din("conf_dw_w", [NL, 31, W])
    conf_dw_b = din("conf_dw_b", [NL, W])
    conf_ln_g = din("conf_ln_g", [NL, W])
    conf_ln_b = din("conf_ln_b", [NL, W])
    sconv_w = din("sconv_w", [NL, 3, W])
    w_branch = din("w_branch", [NL, 3, W, D])
    w_gate = din("w_gate", [NL, D, 3 * D])
    b_gate = din("b_gate", [NL, 3 * D])
    w_out = din("w_out", [NL, D, D])
    w_rg = din("w_router_group", [NL, D, 4])
    b_rg = din("b_router_group", [NL, 4])
    w_re = din("w_router_expert", [NL, D, NE])
    b_re = din("b_router_expert", [NL, NE])
    w_eg = din("w_exp_gate", [NL, NE, D, W])
    w_eu = din("w_exp_up", [NL, NE, D, W])
    w_ed = din("w_exp_down", [NL, NE, W, D])
    fin_g = din("final_norm_g", [D])
    out = nc.dram_tensor("out", [S, D], F32, kind="ExternalOutput").ap()
    mod_d = nc.dram_tensor("mod_d", [6 * D], F32, kind="Internal").ap()
    xb_d = nc.dram_tensor("xb_d", [CAP, D], BF16, kind="Internal").ap()
    yb_d = nc.dram_tensor("yb_d", [CAP, D], F32, kind="Internal").ap()
    tap_aps = {}

    def tap_out(name, shape, dt=F32):
        tap_aps[name] = nc.dram_tensor("tap_" + name, list(shape), dt, kind="ExternalOutput").ap()
        return tap_aps[name]

    st = ExitStack()
    P = Prog(nc)

    def stop_if(name):
        if ('stop:' + name) in taps:
            P.barrier()
            P.emit(st)
            st.close()
            raise _Stop((nc, list(tap_aps.keys())))
    A = Arena(nc)
    psA = st.enter_context(nc.psum_tensor("psA", [128, 2048], F32))
    psB = st.enter_context(nc.psum_tensor("psB", [128, 1024], F32))
    psT = st.enter_context(nc.psum_tensor("psT", [128, 2, 1024], BF16))

    def V(fn, reads, writes):
        return P.op('dve', fn, reads, writes)

    def Ac(fn, reads, writes):
        return P.op('act', fn, reads, writes)

    def G(fn, reads, writes):
        return P.op('pool', fn, reads, writes)

    def MM(o, lhsT, rhs, start, stop, reads, writes):
        return P.op('pe', _I('matmul', o, lhsT=lhsT, rhs=rhs, start=start, stop=stop), reads, writes)

    def TR(o, in_, ident, reads, writes):
        return P.op('pe', _I('transpose', o, in_, ident), reads, writes)

    def LD(o, i, reads=(), writes=(), q='sp'):
        return P.dma(q, _I('dma_start', out=o, in_=i), reads, writes)

    def LDs(o, i, reads=(), writes=(), q='sp'):
        return P.dma(q, _I('dma_start', out=o, in_=i, allow_slow_non_contiguous=True), reads, writes)

    ident_f = A.alloc("ident_f", [128, 128], F32)
    ident_b = A.alloc("ident_b", [128, 128], BF16)
    ones_d = A.alloc("ones_d", [128, 128], BF16)
    ones1 = A.alloc("ones1", [128, 128], BF16)
    ustrict = A.alloc("ustrict", [128, 128], BF16)
    iota_t = A.alloc("iota_t", [128, S], F32)
    eps_t = A.alloc("eps_t", [128, 1], F32)
    pi_t = A.alloc("pi_t", [128, 1], F32)
    hpi_t = A.alloc("hpi_t", [128, 1], F32)
    condb = A.alloc("condb", [128, 8], BF16)
    b128 = A.alloc("b128", [128, NBLK], F32)
    ones_row = A.alloc("ones_row", [128, NE], F32)
    iota_e = A.alloc("iota_e", [128, NE], F32)

    G(_I('memset', ident_f[:], 0.0), [], ['ident_f'])
    G(_I('affine_select', out=ident_f[:], in_=ident_f[:], pattern=[[-1, 128]], compare_op=ALU.not_equal,
                                fill=1.0, base=0, channel_multiplier=1), [], ['ident_f'])
    V(_I('tensor_copy', out=ident_b[:], in_=ident_f[:]), ['ident_f'], ['ident_b'])
    V(_I('memset', ones_d[:], 1.0 / 512.0), [], ['ones_d'])
    V(_I('memset', ones1[:], 1.0), [], ['ones1'])
    G(_I('memset', ustrict[:], 1.0), [], ['ustrict'])
    G(_I('affine_select', out=ustrict[:], in_=ustrict[:], pattern=[[1, 128]], compare_op=ALU.is_gt,
                                fill=0.0, base=0, channel_multiplier=-1), [], ['ustrict'])
    G(_I('iota', iota_t[:], pattern=[[1, S]], base=0, channel_multiplier=0,
                       allow_small_or_imprecise_dtypes=True), [], ['iota_t'])
    G(_I('iota', b128[:], pattern=[[128, NBLK]], base=0, channel_multiplier=0,
                       allow_small_or_imprecise_dtypes=True), [], ['b128'])
    G(_I('iota', iota_e[:], pattern=[[1, NE]], base=0, channel_multiplier=0,
                       allow_small_or_imprecise_dtypes=True), [], ['iota_e'])
    V(_I('memset', eps_t[:], EPS), [], ['eps_t'])
    V(_I('memset', pi_t[:], math.pi), [], ['pi_t'])
    V(_I('memset', hpi_t[:], math.pi / 2), [], ['hpi_t'])
    V(_I('memset', ones_row[:], 1.0), [], ['ones_row'])
    m0 = A.mark()
    c_sb = A.alloc("c_sb", [128, 8], F32)
    LDs(c_sb[:], c_in.rearrange("(k p) -> p k", p=128), [], ['c_sb'])
    Ac(_I('activation', out=condb[:], in_=c_sb[:], func=AF.Silu), ['c_sb'], ['condb'])
    P.barrier()
    A.release(m0)
    base_mark = A.mark()

    def rstd_from_ss(ss_ap, rstd_ap, scale, keys_r, keys_w):
        Ac(_I('activation', out=rstd_ap, in_=ss_ap, func=AF.Sqrt, bias=eps_t[:, 0:1], scale=scale), keys_r, keys_w)
        V(_I('reciprocal', out=rstd_ap, in_=rstd_ap), keys_w, keys_w)

    INV2PI = 1.0 / TWO_PI

    MAGIC = 12582912.0

    def range_reduce(x_ap, n_ap, m_ap, rk, wk_):
        V(_I('tensor_scalar', out=m_ap, in0=x_ap, scalar1=INV2PI, scalar2=MAGIC, op0=ALU.mult, op1=ALU.add), rk, wk_)
        V(_I('tensor_scalar', out=m_ap, in0=m_ap, scalar1=-MAGIC, scalar2=-TWO_PI, op0=ALU.add, op1=ALU.mult), wk_, wk_)
        V(_I('tensor_tensor', out=x_ap, in0=x_ap, in1=m_ap, op=ALU.add), wk_, wk_)

    for l in range(n_layers):
        xsrc = x_in if l == 0 else out
        A.release(base_mark)
        mk = A.mark()
        barow = A.alloc("barow", [1, 6 * D], F32)
        LD(barow[0:1, :], b_ada[l:l + 1, :], [], ['barow'])
        wadab = [A.alloc("wada%d" % i, [128, 8, 512], BF16) for i in range(2)]
        mtmp = [A.alloc("mtmp%d" % i, [1, 512], F32) for i in range(2)]
        for n in range(12):
            wb = wadab[n % 2]
            LD(wb[:], w_ada[l, :, n * 512:(n + 1) * 512].rearrange("(k p) f -> p k f", p=128), [], [('wada', n % 2)], q='pool')
            for k in range(8):
                MM(psA[0:1, (n % 2) * 512:(n % 2) * 512 + 512], condb[:, k:k + 1], wb[:, k, :], k == 0, k == 7,
                   ['condb', ('wada', n % 2)], ['pA%d' % (n % 2)])
            mt = mtmp[n % 2]
            V(_I('tensor_tensor', out=mt[0:1, :], in0=psA[0:1, (n % 2) * 512:(n % 2) * 512 + 512],
                                                    in1=barow[0:1, n * 512:(n + 1) * 512], op=ALU.add),
              ['pA%d' % (n % 2), 'barow'], [('mtmp', n % 2)])
            LD(mod_d[n * 512:(n + 1) * 512].rearrange("(o f) -> o f", o=1), mt[0:1, :], [('mtmp', n % 2)], [('mod_d', n)])
        P.barrier()
        A.release(mk)

        def load_rows(jA, jB, gsrc):
            rowA = A.alloc("rowA", [128, D], F32)
            rowB = A.alloc("rowB", [128, D], F32)
            gt_ = A.alloc("gt_", [128, D], F32)
            LD(rowA[:], mod_d[jA * D:(jA + 1) * D].partition_broadcast(128), [], ['rowA'])
            LD(rowB[:], mod_d[jB * D:(jB + 1) * D].partition_broadcast(128), [], ['rowB'])
            LD(gt_[:], gsrc[l].partition_broadcast(128), [], ['gt_'])
            V(_I('scalar_tensor_tensor', out=rowA[:], in0=rowA[:], scalar=1.0, in1=gt_[:], op0=ALU.add, op1=ALU.mult),
              ['rowA', 'gt_'], ['rowA'])
            return rowA, rowB

        def load_row(j):
            rowG = A.alloc("rowG", [128, D], F32)
            LD(rowG[:], mod_d[j * D:(j + 1) * D].partition_broadcast(128), [], ['rowG'])
            return rowG

        hT = A.alloc("hT", [128, 8, S], BF16)
        ys = [None, None, None]
        ys[0] = A.alloc("ys0", [128, 4, S], BF16)
        ys1_mark = A.mark()
        ys[1] = A.alloc("ys1", [128, 4, S], BF16)
        ys[2] = A.alloc("ys2", [128, 4, S], BF16)
        mixer_mark = A.mark()
        rowA, rowB = load_rows(1, 0, norm_mix_g)
        xt = [A.alloc("xt%d" % i, [128, D], F32) for i in range(2)]
        junk = A.alloc("junk", [128, D], BF16)
        htmp = A.alloc("htmp", [128, D], F32)
        hb = [A.alloc("hb%d" % i, [128, D], BF16) for i in range(2)]
        ss = A.alloc("ss", [128, NT], F32)
        rstd = A.alloc("rstd", [128, NT], F32)
        V(_I('memset', ss[:], 0.0), [], ['ss'])
        for t in range(NT):
            x_ = xt[t % 2]
            LD(x_[:], xsrc[t * 128:(t + 1) * 128, :], [('xres', t)], [('xt', t % 2)])
            Ac(_I('activation', out=junk[:], in_=x_[:], func=AF.Square, accum_out=ss[:, t:t + 1]),
               [('xt', t % 2), 'ss'], ['junk', ('ss', t)])
            rstd_from_ss(ss[:, t:t + 1], rstd[:, t:t + 1], 1.0 / D, [('ss', t)], [('rstd', t)])
            V(_I('scalar_tensor_tensor', out=htmp[:], in0=x_[:], scalar=rstd[:, t:t + 1], in1=rowA[:],
                                                           op0=ALU.mult, op1=ALU.mult), [('xt', t % 2), ('rstd', t), 'rowA'], ['htmp'])
            hb_ = hb[t % 2]
            V(_I('tensor_tensor', out=hb_[:], in0=htmp[:], in1=rowB[:], op=ALU.add),
              ['htmp', 'rowB'], [('hb', t % 2)])
            for k in range(8):
                TR(psT[:, t % 2, k * 128:(k + 1) * 128], hb_[:, k * 128:(k + 1) * 128], ident_b[:],
                   [('hb', t % 2), 'ident_b'], [('psT', t % 2)])
            Ac(_I('activation', out=hT[:, :, t * 128:(t + 1) * 128],
                                           in_=psT[:, t % 2, :].rearrange("p (k m) -> p k m", k=8), func=AF.Copy),
               [('psT', t % 2)], [('hT', t)])
        P.barrier()
        if 'n1dbg' not in taps:
            A.release(mixer_mark)
        if 'hT' in taps and l == 0:
            tp = tap_out('hT', [8, 128, S], BF16)
            LD(tp.rearrange("k p s -> p k s"), hT[:], [], ['tap_hT'])
        if 'n1dbg' in taps and l == 0:
            LD(tap_out('ss', [128, NT]), ss[:], [], ['tap_ss'])
            LD(tap_out('rstd', [128, NT]), rstd[:], [], ['tap_rstd'])
            LD(tap_out('rowA', [128, D]), rowA[:], [], ['tap_rowA'])
            LD(tap_out('rowB', [128, D]), rowB[:], [], ['tap_rowB'])
            LD(tap_out('hb1', [128, D], BF16), hb[1][:], [], ['tap_hb1'])
            LD(tap_out('modd', [6 * D]), mod_d, [], ['tap_modd'])
            P.barrier()
            P.emit(st)
            st.close()
            return nc, list(tap_aps.keys())

        def load_win(dst, j, key):
            LD(dst[:], w_in[l, :, j * 512:(j + 1) * 512].rearrange("(k p) f -> p k f", p=128), [], [key], q='pool')

        def proj_in(wt, wkey, ft, tc, ps_ap, pkey):
            for k in range(8):
                MM(ps_ap, wt[:, k, ft * 128:(ft + 1) * 128], hT[:, k, tc * 512:(tc + 1) * 512], k == 0, k == 7,
                   [wkey], [pkey])

        A.release(ys1_mark)
        uT = A.alloc("uT", [128, 4, S], BF16)
        rho = A.alloc("rho", [128, 16], F32)
        th = A.alloc("th", [128, 16], F32)
        Bpad = [A.alloc("Bpad%d" % c, [128, 16, 128], BF16) for c in range(2)]
        Cpad = [A.alloc("Cpad%d" % c, [128, 16, 128], BF16) for c in range(3)]
        Dd = A.alloc("Dd", [128, 4, 128], BF16)
        mk2 = A.mark()
        wu = A.alloc("wu", [128, 8, 512], BF16)
        load_win(wu, 0, 'wu')
        for ft in range(4):
            for tc in range(4):
                proj_in(wu, 'wu', ft, tc, psA[:, tc * 512:(tc + 1) * 512], 'pA%d' % tc)
                Ac(_I('activation', out=uT[:, ft, tc * 512:(tc + 1) * 512], in_=psA[:, tc * 512:(tc + 1) * 512],
                                                        func=AF.Copy), ['pA%d' % tc], [('uT', ft)])
        lr = A.alloc("lr", [128, 16], F32)
        li = A.alloc("li", [128, 16], F32)
        stp = A.alloc("stp", [128, 16], F32)
        LDs(lr[:], lam_re[l].rearrange("(q g) p -> (g p) q", g=2), [], ['lr'])
        LDs(li[:], lam_im[l].rearrange("(q g) p -> (g p) q", g=2), [], ['li'])
        ls2 = log_step[l].rearrange("(q g) -> g q", g=2)
        for g in range(2):
            LDs(stp[g * 64:(g + 1) * 64, :], ls2[g:g + 1, :].to_broadcast([64, 16]), [], [('stp', g)])
        kr = A.alloc("kr", [128, 16], F32)
        ki = A.alloc("ki", [128, 16], F32)
        sp_t = [A.alloc("spt%d" % i, [128, 16], F32) for i in range(6)]
        Ac(_I('activation', out=stp[:], in_=stp[:], func=AF.Exp), [('stp', 0), ('stp', 1)], ['stp'])
        V(_I('tensor_tensor', out=rho[:], in0=lr[:], in1=stp[:], op=ALU.mult), ['lr', 'stp'], ['rho'])
        Ac(_I('activation', out=rho[:], in_=rho[:], func=AF.Exp), ['rho'], ['rho'])
        V(_I('tensor_tensor', out=th[:], in0=li[:], in1=stp[:], op=ALU.mult), ['li', 'stp'], ['th'])
        ysn, ycs, sn, cs, den, tmpa = sp_t
        thn = A.alloc("thn", [128, 16], I32)
        V(_I('tensor_scalar_add', out=ysn[:], in0=th[:], scalar1=TWO_PI), ['th'], ['ysn'])
        range_reduce(ysn[:], thn[:], tmpa[:], ['ysn'], ['ysn', 'thn', 'tmpa'])
        Ac(_I('activation', out=sn[:], in_=ysn[:], func=AF.Sin), ['ysn'], ['sn'])
        Ac(_I('activation', out=ycs[:], in_=ysn[:], func=AF.Abs), ['ysn'], ['ycs'])
        Ac(_I('activation', out=cs[:], in_=ycs[:], func=AF.Sin, bias=hpi_t[:, 0:1], scale=-1.0), ['ycs'], ['cs'])
        V(_I('tensor_tensor', out=cs[:], in0=cs[:], in1=rho[:], op=ALU.mult), ['cs', 'rho'], ['cs'])
        V(_I('tensor_scalar_add', out=cs[:], in0=cs[:], scalar1=-1.0), ['cs'], ['cs'])
        V(_I('tensor_tensor', out=sn[:], in0=sn[:], in1=rho[:], op=ALU.mult), ['sn', 'rho'], ['sn'])
        V(_I('tensor_tensor', out=den[:], in0=lr[:], in1=lr[:], op=ALU.mult), ['lr'], ['den'])
        V(_I('tensor_tensor', out=tmpa[:], in0=li[:], in1=li[:], op=ALU.mult), ['li'], ['tmpa'])
        V(_I('tensor_tensor', out=den[:], in0=den[:], in1=tmpa[:], op=ALU.add), ['den', 'tmpa'], ['den'])
        V(_I('reciprocal', out=den[:], in_=den[:]), ['den'], ['den'])
        V(_I('tensor_tensor', out=kr[:], in0=cs[:], in1=lr[:], op=ALU.mult), ['cs', 'lr'], ['kr'])
        V(_I('tensor_tensor', out=tmpa[:], in0=sn[:], in1=li[:], op=ALU.mult), ['sn', 'li'], ['tmpa'])
        V(_I('tensor_tensor', out=kr[:], in0=kr[:], in1=tmpa[:], op=ALU.add), ['kr', 'tmpa'], ['kr'])
        V(_I('tensor_tensor', out=kr[:], in0=kr[:], in1=den[:], op=ALU.mult), ['kr', 'den'], ['kr'])
        V(_I('tensor_tensor', out=ki[:], in0=sn[:], in1=lr[:], op=ALU.mult), ['sn', 'lr'], ['ki'])
        V(_I('tensor_tensor', out=tmpa[:], in0=cs[:], in1=li[:], op=ALU.mult), ['cs', 'li'], ['tmpa'])
        V(_I('tensor_tensor', out=ki[:], in0=ki[:], in1=tmpa[:], op=ALU.subtract), ['ki', 'tmpa'], ['ki'])
        V(_I('tensor_tensor', out=ki[:], in0=ki[:], in1=den[:], op=ALU.mult), ['ki', 'den'], ['ki'])
        braw = [A.alloc("braw%d" % c, [128, 16, 16], F32) for c in range(2)]
        LDs(braw[0][:], ssm_b_re[l].rearrange("(q g) p h -> (g p) q h", g=2), [], ['braw0'])
        LDs(braw[1][:], ssm_b_im[l].rearrange("(q g) p h -> (g p) q h", g=2), [], ['braw1'])
        bb = [A.alloc("bb%d" % c, [128, 16, 16], F32) for c in range(2)]
        bt = A.alloc("bt", [128, 16, 16], F32)
        krb = kr[:, :].unsqueeze(2).to_broadcast([128, 16, 16])
        kib = ki[:, :].unsqueeze(2).to_broadcast([128, 16, 16])
        V(_I('tensor_tensor', out=bb[0][:], in0=braw[0][:], in1=krb, op=ALU.mult), ['braw0', 'kr'], ['bb0'])
        V(_I('tensor_tensor', out=bt[:], in0=braw[1][:], in1=kib, op=ALU.mult), ['braw1', 'ki'], ['bt'])
        V(_I('tensor_tensor', out=bb[0][:], in0=bb[0][:], in1=bt[:], op=ALU.subtract), ['bb0', 'bt'], ['bb0'])
        V(_I('tensor_tensor', out=bb[1][:], in0=braw[1][:], in1=krb, op=ALU.mult), ['braw1', 'kr'], ['bb1'])
        V(_I('tensor_tensor', out=bt[:], in0=braw[0][:], in1=kib, op=ALU.mult), ['braw0', 'ki'], ['bt'])
        V(_I('tensor_tensor', out=bb[1][:], in0=bb[1][:], in1=bt[:], op=ALU.add), ['bb1', 'bt'], ['bb1'])
        nat = A.alloc("nat", [128, 16, 128], F32)
        natv = nat[:].rearrange("p (qq r) (r2 g2 h) -> p qq r r2 g2 h", r=4, r2=4, g2=2)
        for c in range(2):
            V(_I('memset', nat[:], 0.0), [], ['nat'])
            bbv = bb[c][:].rearrange("p (qq r) h -> p qq r h", r=4)
            for r in range(4):
                for g in range(2):
                    V(_I('tensor_copy', out=natv[g * 64:(g + 1) * 64, :, r, r, g, :],
                                                                 in_=bbv[g * 64:(g + 1) * 64, :, r, :]),
                      ['bb%d' % c, 'nat'], ['nat'])
            for q in range(16):
                TR(psB[:, (q % 8) * 128:(q % 8 + 1) * 128], nat[:, q, :], ident_f[:], ['nat', 'ident_f'], ['pB%d' % ((q % 8) // 4)])
                if q % 8 == 7:
                    hq = q // 8
                    V(_I('tensor_copy', out=Bpad[c][:, hq * 8:(hq + 1) * 8, :],
                                                          in_=psB[:].rearrange("p (q m) -> p q m", q=8)),
                      ['pB0', 'pB1'], ['Bpad'])
        cnat = A.alloc("cnat", [128, 16, 128], F32)
        cnv = cnat[:].rearrange("p (ft r) (g s) -> p ft r g s", r=4, g=2)
        for c, (src, sgns) in enumerate(((ssm_c_re, ((0, 1.0), (1, -1.0))), (ssm_c_im, ((2, -1.0),)))):
            V(_I('memset', cnat[:], 0.0), [], ['cnat'])
            srcv = src[l].rearrange("(ft r g) h s -> r g h ft s", r=4, g=2)
            for r in range(4):
                for g in range(2):
                    p0 = 32 * r + 16 * g
                    LDs(cnv[p0:p0 + 16, :, r, g, :], srcv[r, g], ['cnat'], [('cnat', r, g)])
            ckeys = ['cnat'] + [('cnat', r, g) for r in range(4) for g in range(2)]
            for q in range(16):
                TR(psB[:, (q % 8) * 128:(q % 8 + 1) * 128], cnat[:, q, :], ident_f[:], ckeys + ['ident_f'], ['pB%d' % ((q % 8) // 4)])
                if q % 8 == 7:
                    hq = q // 8
                    for (ci, sg) in sgns:
                        Ac(_I('activation', out=Cpad[ci][:, hq * 8:(hq + 1) * 8, :],
                                                                       in_=psB[:].rearrange("p (q m) -> p q m", q=8),
                                                                       func=AF.Copy, scale=sg),
                           ['pB0', 'pB1'], ['Cpad'])
        dcol = A.alloc("dcol", [128, 4], F32)
        LDs(dcol[:], ssm_d[l].rearrange("(ft p) -> p ft", p=128), [], ['dcol'])
        for ft in range(4):
            V(_I('tensor_scalar_mul', out=Dd[:, ft, :], in0=ident_f[:], scalar1=dcol[:, ft:ft + 1]),
              ['dcol', 'ident_f'], ['Dd'])
        P.barrier()
        A.release(mk2)
        if 'uT' in taps and l == 0:
            tp = tap_out('uT', [4, 128, S], BF16)
            LD(tp.rearrange("k p s -> p k s"), uT[:], [], ['tap_uT'])
        if 'ssmw' in taps and l == 0:
            LD(tap_out('Bpad0', [128, 16, 128], BF16), Bpad[0][:], [], ['tap_b0'])
            LD(tap_out('Bpad1', [128, 16, 128], BF16), Bpad[1][:], [], ['tap_b1'])
            LD(tap_out('Cpad0', [128, 16, 128], BF16), Cpad[0][:], [], ['tap_c0'])
            LD(tap_out('Cpad2', [128, 16, 128], BF16), Cpad[2][:], [], ['tap_c2'])
            LD(tap_out('rho', [128, 16]), rho[:], [], ['tap_rho'])
            LD(tap_out('th', [128, 16]), th[:], [], ['tap_th'])
        if l == 0:
            stop_if('uT')
        HS = 1024
        wglu = A.alloc("wglu", [128, 4, 512], BF16)
        LD(wglu[:], w_glu[l].rearrange("(k p) f -> p k f", p=128), [], ['wglu'], q='pool')
        ygb = A.alloc("ygb", [128, 4, HS], BF16)
        carry = A.alloc("carry", [128, 16, 2], F32)
        V(_I('memset', carry[:], 0.0), [], ['carry'])
        nset = 2
        Ct = [A.alloc("Ct%d" % i, [128, HS], F32) for i in range(nset)]
        St = [A.alloc("St%d" % i, [128, HS], F32) for i in range(nset)]
        t1 = [A.alloc("t1%d" % i, [128, HS], F32) for i in range(nset)]
        t2 = [A.alloc("t2%d" % i, [128, HS], F32) for i in range(nset)]
        qre = [A.alloc("qre%d" % i, [128, HS], F32) for i in range(nset)]
        qim = [A.alloc("qim%d" % i, [128, HS], F32) for i in range(nset)]
        Pp = [A.alloc("Pp%d" % i, [128, 4, HS], BF16) for i in range(nset)]
        ph = A.alloc("ph", [128, HS], F32)
        pm = A.alloc("pm", [128, HS], F32)
        bur = [A.alloc("bur%d" % i, [128, HS], F32) for i in range(nset)]
        bui = [A.alloc("bui%d" % i, [128, HS], F32) for i in range(nset)]
        ysb = A.alloc("ysb", [128, HS], F32)
        gtm = A.alloc("gtm", [128, HS], F32)
        sgt = A.alloc("sgt", [128, 512], F32)
        YK = ['pB0', 'pB1']
        iters = [(half, ft, r) for half in range(2) for ft in range(4) for r in range(4)]

        def stage1a(i):
            half, ft, r = iters[i]
            q = 4 * ft + r
            s_ = i % nset
            t0 = half * HS
            k_ = lambda nm: (nm, s_)
            for c in range(2):
                for tc in range(2):
                    MM(psA[:, c * 1024 + tc * 512: c * 1024 + (tc + 1) * 512], Bpad[c][:, q, :],
                       uT[:, ft, t0 + tc * 512: t0 + (tc + 1) * 512], True, True,
                       ['Bpad', ('uT', ft)], ['pA%d' % (c * 2 + tc)])
            Ac(_I('activation', out=bur[s_][:], in_=psA[:, 0:1024], func=AF.Copy), ['pA0', 'pA1'], [k_('bur')])
            Ac(_I('activation', out=bui[s_][:], in_=psA[:, 1024:2048], func=AF.Copy), ['pA2', 'pA3'], [k_('bui')])

        def stage1b(i):
            half, ft, r = iters[i]
            q = 4 * ft + r
            s_ = i % nset
            t0 = half * HS
            k_ = lambda nm: (nm, s_)
            Ac(_I('activation', out=ph[:], in_=iota_t[:, t0:t0 + HS], func=AF.Identity,
                  bias=8.0 * math.pi, scale=th[:, q:q + 1]), ['iota_t', 'th'], ['ph'])
            range_reduce(ph[:], None, pm[:], ['ph'], ['ph', 'pm'])
            Ac(_I('activation', out=St[s_][:], in_=ph[:], func=AF.Sin), ['ph'], [k_('St')])
            Ac(_I('activation', out=pm[:], in_=ph[:], func=AF.Abs), ['ph'], ['pm'])
            Ac(_I('activation', out=Ct[s_][:], in_=pm[:], func=AF.Sin, bias=hpi_t[:, 0:1], scale=-1.0), ['pm'], [k_('Ct')])

        def stage2(i):
            half, ft, r = iters[i]
            q = 4 * ft + r
            s_ = i % nset
            k_ = lambda nm: (nm, s_)
            bure, buim = bur[s_][:], bui[s_][:]
            KRE, KIM = [k_('bur')], [k_('bui')]
            V(_I('tensor_tensor', out=t1[s_][:], in0=bure, in1=Ct[s_][:], op=ALU.mult), KRE + [k_('Ct')], [k_('t1')])
            V(_I('tensor_tensor', out=t2[s_][:], in0=buim, in1=St[s_][:], op=ALU.mult), KIM + [k_('St')], [k_('t2')])
            V(_I('tensor_tensor', out=qre[s_][:], in0=t1[s_][:], in1=t2[s_][:], op=ALU.add), [k_('t1'), k_('t2')], [k_('qre')])
            V(_I('tensor_tensor', out=t1[s_][:], in0=buim, in1=Ct[s_][:], op=ALU.mult), KIM + [k_('Ct')], [k_('t1')])
            V(_I('tensor_tensor', out=t2[s_][:], in0=bure, in1=St[s_][:], op=ALU.mult), KRE + [k_('St')], [k_('t2')])
            V(_I('tensor_tensor', out=qim[s_][:], in0=t1[s_][:], in1=t2[s_][:], op=ALU.subtract), [k_('t1'), k_('t2')], [k_('qim')])
            V(_I('tensor_tensor_scan', out=t1[s_][:], data0=rho[:, q:q + 1].to_broadcast([128, HS]), data1=qre[s_][:],
                 initial=carry[:, q, 0:1], op0=ALU.mult, op1=ALU.add), [k_('qre'), 'rho', ('carry', q)], [k_('t1')])
            V(_I('tensor_tensor_scan', out=t2[s_][:], data0=rho[:, q:q + 1].to_broadcast([128, HS]), data1=qim[s_][:],
                 initial=carry[:, q, 1:2], op0=ALU.mult, op1=ALU.add), [k_('qim'), 'rho', ('carry', q)], [k_('t2')])
            if half == 0:
                Ac(_I('activation', out=carry[:, q, 0:1], in_=t1[s_][:, HS - 1:HS], func=AF.Copy), [k_('t1')], [('carry', q)])
                Ac(_I('activation', out=carry[:, q, 1:2], in_=t2[s_][:, HS - 1:HS], func=AF.Copy), [k_('t2')], [('carry', q)])

        def stage3(i):
            half, ft, r = iters[i]
            q = 4 * ft + r
            s_ = i % nset
            t0 = half * HS
            k_ = lambda nm: (nm, s_)
            for pi_, (tt_, zz) in enumerate(((Ct, t1), (St, t2), (St, t1), (Ct, t2))):
                G(_I('tensor_tensor', out=Pp[s_][:, pi_, :], in0=tt_[s_][:], in1=zz[s_][:], op=ALU.mult),
                  [k_('Ct'), k_('St'), k_('t1'), k_('t2')], [(k_('Pp'), pi_)])
            for tc in range(2):
                for pi_, ci in enumerate((0, 1, 2, 2)):
                    MM(psB[:, tc * 512:(tc + 1) * 512], Cpad[ci][:, q, :], Pp[s_][:, pi_, tc * 512:(tc + 1) * 512],
                       (r == 0 and pi_ == 0), False, ['Cpad', (k_('Pp'), pi_)], ['pB%d' % tc])
            if r != 3:
                return
            for tc in range(2):
                MM(psB[:, tc * 512:(tc + 1) * 512], Dd[:, ft, :], uT[:, ft, t0 + tc * 512:t0 + (tc + 1) * 512], False, True,
                   ['Dd', ('uT', ft)], ['pB%d' % tc])
            Ac(_I('activation', out=ysb[:], in_=psB[:], func=AF.Copy), YK, ['ysb'])
            V(_I('tensor_tensor', out=gtm[:], in0=ysb[:], in1=ysb[:], op=ALU.mult), ['ysb'], ['gtm'])
            V(_I('tensor_scalar', out=gtm[:], in0=gtm[:], scalar1=0.044715, scalar2=1.0, op0=ALU.mult, op1=ALU.add), ['gtm'], ['gtm'])
            V(_I('tensor_tensor', out=gtm[:], in0=gtm[:], in1=ysb[:], op=ALU.mult), ['gtm', 'ysb'], ['gtm'])
            Ac(_I('activation', out=gtm[:], in_=gtm[:], func=AF.Sigmoid, scale=1.5957691216057308), ['gtm'], ['gtm'])
            V(_I('tensor_tensor', out=ygb[:, ft, :], in0=gtm[:], in1=ysb[:], op=ALU.mult), ['gtm', 'ysb'], [('ygb', ft)])
            if ft != 3:
                return
            for fo in range(4):
                for tc in range(2):
                    for k in range(4):
                        MM(psB[:, tc * 512:(tc + 1) * 512], wglu[:, k, fo * 128:(fo + 1) * 128], ygb[:, k, tc * 512:(tc + 1) * 512],
                           k == 0, k == 3, ['wglu'] + [('ygb', f) for f in range(4)], ['pB%d' % tc])
                    Ac(_I('activation', out=sgt[:], in_=psB[:, tc * 512:(tc + 1) * 512], func=AF.Sigmoid), ['pB%d' % tc], ['sgt'])
                    V(_I('tensor_tensor', out=ys[0][:, fo, t0 + tc * 512:t0 + (tc + 1) * 512], in0=sgt[:],
                         in1=ygb[:, fo, tc * 512:(tc + 1) * 512], op=ALU.mult), ['sgt', ('ygb', fo)], [('ys', 0)])

        stage1a(0)
        stage1b(0)
        for i in range(len(iters)):
            if i + 1 < len(iters):
                stage1a(i + 1)
                stage1b(i + 1)
            stage2(i)
            stage3(i)
        P.barrier()
        A.release(mixer_mark)
        if 'ys0' in taps and l == 0:
            tp = tap_out('ys0', [4, 128, S], BF16)
            LD(tp.rearrange("k p s -> p k s"), ys[0][:], [], ['tap_ys0'])

        if l == 0:
            stop_if('ys0')
        mk = A.mark()
        wv = A.alloc("wv", [128, 8, 512], BF16)
        wg = A.alloc("wg", [128, 8, 512], BF16)
        load_win(wv, 1, 'wv')
        load_win(wg, 2, 'wg')
        cw = A.alloc("cw", [128, 4, 31], F32)
        cbias = A.alloc("cbias", [128, 4], F32)
        lng = A.alloc("lng", [128, 4], F32)
        lnb = A.alloc("lnb", [128, 4], F32)
        for ft in range(4):
            LDs(cw[:, ft, :], conf_dw_w[l][:, ft * 128:(ft + 1) * 128].rearrange("k p -> p k"), [], ['cw'])
        LDs(cbias[:], conf_dw_b[l].rearrange("(ft p) -> p ft", p=128), [], ['cbias'])
        LDs(lng[:], conf_ln_g[l].rearrange("(ft p) -> p ft", p=128), [], ['lng'])
        LDs(lnb[:], conf_ln_b[l].rearrange("(ft p) -> p ft", p=128), [], ['lnb'])
        zt = A.alloc("zt", [128, 32 + S], F32)
        cacc = A.alloc("cacc", [128, 4, S], F32)
        sgc = [A.alloc("sgc%d" % i, [128, 512], F32) for i in range(2)]
        cbt = [A.alloc("cbt%d" % i, [128, 512], BF16) for i in range(2)]
        cst = [A.alloc("cst%d" % i, [128, 512], BF16) for i in range(2)]
        mean_sb = A.alloc("mean_sb", [128, S], F32)
        rstd_sb = A.alloc("rstd_sb", [128, S], F32)
        V(_I('memset', zt[:, 0:32], 0.0), [], ['zt'])
        for ft in range(4):
            for tc in range(4):
                proj_in(wv, 'wv', ft, tc, psA[:, (tc % 2) * 512:(tc % 2 + 1) * 512], ('psA', 'v', tc % 2))
                proj_in(wg, 'wg', ft, tc, psA[:, 1024 + (tc % 2) * 512:1024 + (tc % 2 + 1) * 512], ('psA', 'g', tc % 2))
                Ac(_I('activation', out=sgc[tc % 2][:], in_=psA[:, 1024 + (tc % 2) * 512:1024 + (tc % 2 + 1) * 512],
                                                 func=AF.Sigmoid), [('psA', 'g', tc % 2)], [('sgc', tc % 2)])
                V(_I('tensor_tensor', out=zt[:, 32 + tc * 512:32 + (tc + 1) * 512],
                                                   in0=psA[:, (tc % 2) * 512:(tc % 2 + 1) * 512], in1=sgc[tc % 2][:], op=ALU.mult),
                  [('psA', 'v', tc % 2), ('sgc', tc % 2)], ['zt'])
            V(_I('tensor_scalar', out=cacc[:, ft, :], in0=zt[:, 32:32 + S], scalar1=cw[:, ft, 30:31],
                                               scalar2=cbias[:, ft:ft + 1], op0=ALU.mult, op1=ALU.add),
              ['zt', 'cw', 'cbias'], [('cacc', ft)])
            for k in range(30):
                V(_I('scalar_tensor_tensor', out=cacc[:, ft, :], in0=zt[:, 2 + k:2 + k + S], scalar=cw[:, ft, k:k + 1],
                                                               in1=cacc[:, ft, :], op0=ALU.mult, op1=ALU.add),
                  ['zt', 'cw'], [('cacc', ft)])
        for tc in range(4):
            for ft in range(4):
                i_ = (tc * 4 + ft) % 2
                Ac(_I('activation', out=cbt[i_][:], in_=cacc[:, ft, tc * 512:(tc + 1) * 512], func=AF.Copy),
                   [('cacc', ft)], [('cbt', i_)])
                Ac(_I('activation', out=cst[i_][:], in_=cacc[:, ft, tc * 512:(tc + 1) * 512], func=AF.Square),
                   [('cacc', ft)], [('cst', i_)])
                MM(psB[:, 0:512], ones_d[:], cbt[i_][:], ft == 0, ft == 3, ['ones_d', ('cbt', i_)], [('psB', 'mean')])
                MM(psB[:, 512:1024], ones_d[:], cst[i_][:], ft == 0, ft == 3, ['ones_d', ('cst', i_)], [('psB', 'msq')])
            sl = slice(tc * 512, (tc + 1) * 512)
            Ac(_I('activation', out=mean_sb[:, sl], in_=psB[:, 0:512], func=AF.Copy), [('psB', 'mean')], [('mean_sb', tc)])
            V(_I('tensor_tensor', out=rstd_sb[:, sl], in0=mean_sb[:, sl], in1=mean_sb[:, sl], op=ALU.mult),
              [('mean_sb', tc)], [('rstd_sb', tc)])
            V(_I('tensor_tensor', out=rstd_sb[:, sl], in0=psB[:, 512:1024], in1=rstd_sb[:, sl], op=ALU.subtract),
              [('psB', 'msq'), ('rstd_sb', tc)], [('rstd_sb', tc)])
            rstd_from_ss(rstd_sb[:, sl], rstd_sb[:, sl], 1.0, [('rstd_sb', tc)], [('rstd_sb', tc)])
        for ft in range(4):
            V(_I('tensor_tensor', out=cacc[:, ft, :], in0=cacc[:, ft, :], in1=mean_sb[:], op=ALU.subtract),
              [('cacc', ft)] + [('mean_sb', t) for t in range(4)], [('cacc', ft)])
            V(_I('tensor_tensor', out=cacc[:, ft, :], in0=cacc[:, ft, :], in1=rstd_sb[:], op=ALU.mult),
              [('cacc', ft)] + [('rstd_sb', t) for t in range(4)], [('cacc', ft)])
            Ac(_I('activation', out=ys[1][:, ft, :], in_=cacc[:, ft, :], func=AF.Silu, bias=lnb[:, ft:ft + 1],
                                             scale=lng[:, ft:ft + 1]), [('cacc', ft), 'lng', 'lnb'], [('ys', 1)])
        P.barrier()
        A.release(mk)
        if 'ys1' in taps and l == 0:
            tp = tap_out('ys1', [4, 128, S], BF16)
            LD(tp.rearrange("k p s -> p k s"), ys[1][:], [], ['tap_ys1'])

        if l == 0:
            stop_if('ys1')
        mk = A.mark()
        wsb = [A.alloc("wsb%d" % i, [128, 8, 512], BF16) for i in range(3)]
        for i in range(3):
            load_win(wsb[i], 3 + i, ('wsb', i))
        scw = A.alloc("scw", [128, 4, 3], F32)
        for ft in range(4):
            LDs(scw[:, ft, :], sconv_w[l][:, ft * 128:(ft + 1) * 128].rearrange("k p -> p k"), [], ['scw'])
        pt_ = A.alloc("pt_", [128, 2 + S], F32)
        bs = A.alloc("bs", [128, S], F32)
        cs_ = [A.alloc("cs_%d" % i, [128, 512], F32) for i in range(2)]
        sacc = A.alloc("sacc", [128, S], F32)
        V(_I('memset', pt_[:, 0:2], 0.0), [], ['pt_'])
        for ft in range(4):
            for tc in range(4):
                proj_in(wsb[0], ('wsb', 0), ft, tc, psA[:, 0:512], ('psA', 'b'))
                proj_in(wsb[1], ('wsb', 1), ft, tc, psA[:, 512:1024], ('psA', 'c'))
                proj_in(wsb[2], ('wsb', 2), ft, tc, psA[:, 1024:1536], ('psA', 'h'))
                Ac(_I('activation', out=bs[:, tc * 512:(tc + 1) * 512], in_=psA[:, 0:512], func=AF.Copy),
                   [('psA', 'b')], ['bs'])
                Ac(_I('activation', out=cs_[tc % 2][:], in_=psA[:, 512:1024], func=AF.Copy),
                   [('psA', 'c')], [('cs_', tc % 2)])
                V(_I('tensor_tensor', out=pt_[:, 2 + tc * 512:2 + (tc + 1) * 512], in0=psA[:, 1024:1536],
                                                   in1=cs_[tc % 2][:], op=ALU.mult), [('psA', 'h'), ('cs_', tc % 2)], ['pt_'])
            V(_I('tensor_scalar_mul', out=sacc[:], in0=pt_[:, 2:2 + S], scalar1=scw[:, ft, 2:3]), ['pt_', 'scw'], ['sacc'])
            for k in range(2):
                V(_I('scalar_tensor_tensor', out=sacc[:], in0=pt_[:, k:k + S], scalar=scw[:, ft, k:k + 1],
                                                               in1=sacc[:], op0=ALU.mult, op1=ALU.add), ['pt_', 'scw'], ['sacc'])
            V(_I('tensor_tensor', out=ys[2][:, ft, :], in0=sacc[:], in1=bs[:], op=ALU.mult), ['sacc', 'bs'], [('ys', 2)])
        P.barrier()
        A.release(mk)
        if 'ys2' in taps and l == 0:
            tp = tap_out('ys2', [4, 128, S], BF16)
            LD(tp.rearrange("k p s -> p k s"), ys[2][:], [], ['tap_ys2'])

        if l == 0:
            stop_if('ys2')
        mk = A.mark()
        mixT = A.alloc("mixT", [128, 8, S], BF16)
        wgt = [A.alloc("wgt%d" % n, [128, 8, 512], BF16) for n in range(3)]
        wbr = [A.alloc("wbr%d" % n, [128, 4, 512], BF16) for n in range(3)]
        bgc = A.alloc("bgc", [128, 3, 8], F32)
        for n in range(3):
            LDs(bgc[:, n, :], b_gate[l][n * D:(n + 1) * D].rearrange("(dc p) -> p dc", p=128), [], ['bgc'])
        gsb = [A.alloc("gsb%d" % n, [128, 512], F32) for n in range(3)]
        mt1 = A.alloc("mt1", [128, 512], F32)
        mt2 = A.alloc("mt2", [128, 512], F32)
        for dh in range(2):
            for n in range(3):
                LD(wgt[n][:], w_gate[l, :, n * D + dh * 512:n * D + (dh + 1) * 512].rearrange("(k p) f -> p k f", p=128),
                   [], [('wgt', n)], q='pool')
                LD(wbr[n][:], w_branch[l, n, :, dh * 512:(dh + 1) * 512].rearrange("(k p) f -> p k f", p=128),
                   [], [('wbr', n)], q='pool')
            for dcl in range(4):
                dc = dh * 4 + dcl
                for tc in range(4):
                    tsl = slice(tc * 512, (tc + 1) * 512)
                    for n in range(3):
                        gp = psA[:, n * 512:(n + 1) * 512]
                        for k in range(8):
                            MM(gp, wgt[n][:, k, dcl * 128:(dcl + 1) * 128], hT[:, k, tsl], k == 0, k == 7, [('wgt', n)], [('psA', 'g', n)])
                        bp = psA[:, 1536:2048] if n == 0 else psB[:, (n - 1) * 512:n * 512]
                        for k in range(4):
                            MM(bp, wbr[n][:, k, dcl * 128:(dcl + 1) * 128], ys[n][:, k, tsl], k == 0, k == 3, [('wbr', n)], [('ps', 'br', n)])
                        Ac(_I('activation', out=gsb[n][:], in_=gp, func=AF.Sigmoid, bias=bgc[:, n, dc:dc + 1]),
                           [('psA', 'g', n), 'bgc'], [('gsb', n)])
                    bps = [psA[:, 1536:2048], psB[:, 0:512], psB[:, 512:1024]]
                    V(_I('tensor_tensor', out=mt1[:], in0=bps[0], in1=gsb[0][:], op=ALU.mult),
                      [('ps', 'br', 0), ('gsb', 0)], ['mt1'])
                    V(_I('tensor_tensor', out=mt2[:], in0=bps[1], in1=gsb[1][:], op=ALU.mult),
                      [('ps', 'br', 1), ('gsb', 1)], ['mt2'])
                    V(_I('tensor_tensor', out=mt1[:], in0=mt1[:], in1=mt2[:], op=ALU.add), ['mt1', 'mt2'], ['mt1'])
                    V(_I('tensor_tensor', out=mt2[:], in0=bps[2], in1=gsb[2][:], op=ALU.mult),
                      [('ps', 'br', 2), ('gsb', 2)], ['mt2'])
                    V(_I('tensor_tensor', out=mixT[:, dc, tsl], in0=mt1[:], in1=mt2[:], op=ALU.add),
                      ['mt1', 'mt2'], [('mixT', dc)])
        P.barrier()
        mk = A.mark()
        if 'mixT' in taps and l == 0:
            tp = tap_out('mixT', [8, 128, S], BF16)
            LD(tp.rearrange("k p s -> p k s"), mixT[:], [], ['tap_mixT'])
        if l == 0:
            stop_if('mixT')
        rowG = load_row(2)
        wo = A.alloc("wo", [128, 8, D], BF16)
        LD(wo[:], w_out[l].rearrange("(k p) f -> p k f", p=128), [], ['wo'], q='pool')
        xt = [A.alloc("xt%d" % i, [128, D], F32) for i in range(2)]
        xo = [A.alloc("xo%d" % i, [128, D], F32) for i in range(2)]
        for t in range(NT):
            x_ = xt[t % 2]
            xo_ = xo[t % 2]
            LD(x_[:], xsrc[t * 128:(t + 1) * 128, :], [('xres', t)], [('xt', t % 2)])
            for hf in range(2):
                po = psA[:, ((t % 2) * 2 + hf) * 512:((t % 2) * 2 + hf + 1) * 512]
                for k in range(8):
                    MM(po, mixT[:, k, t * 128:(t + 1) * 128], wo[:, k, hf * 512:(hf + 1) * 512], k == 0, k == 7,
                       ['wo'], [('psA', 'o', t % 2, hf)])
                V(_I('tensor_tensor', out=xo_[:, hf * 512:(hf + 1) * 512], in0=po,
                                                                   in1=rowG[:, hf * 512:(hf + 1) * 512], op=ALU.mult),
                  [('psA', 'o', t % 2, hf), 'rowG'], [('xo', t % 2, hf)])
                V(_I('tensor_tensor', out=xo_[:, hf * 512:(hf + 1) * 512], in0=xo_[:, hf * 512:(hf + 1) * 512],
                                                                  in1=x_[:, hf * 512:(hf + 1) * 512], op=ALU.add),
                  [('xo', t % 2, hf), ('xt', t % 2)], [('xo', t % 2, hf)])
            LD(out[t * 128:(t + 1) * 128, :], xo_[:], [('xo', t % 2, 0), ('xo', t % 2, 1)], [('xres', t)])
        P.barrier()
        A.release(mk)
        A.release(base_mark)
        if 'xmix' in taps and l == 0:
            LD(tap_out('xmix', [S, D]), out, [], ['tap_xmix'], q='pool')
            P.barrier()

        if l == 0:
            stop_if('xmix')
        d_i = [A.alloc("d_i%d" % k, [128, NT], I32) for k in range(2)]
        wk = [A.alloc("wk%d" % k, [128, NT], F32) for k in range(2)]
        widx = A.alloc("widx", [128, NBLK], I32)
        widx2 = A.alloc("widx2", [128, NBLK, 4], I32)
        moe_mark = A.mark()
        h2tm = A.alloc("h2tm", [128, NT, D], BF16)
        lgr = A.alloc("lgr", [128, NT, 36], F32)
        mk = A.mark()
        rowA, rowB = load_rows(4, 3, norm_ffn_g)
        xt = [A.alloc("xt%d" % i, [128, D], F32) for i in range(2)]
        junk = A.alloc("junk", [128, D], BF16)
        htmp = A.alloc("htmp", [128, D], F32)
        h2f = [A.alloc("h2f%d" % i, [128, D], F32) for i in range(2)]
        h2T = [A.alloc("h2T%d" % i, [128, 8, 128], F32) for i in range(2)]
        ss = A.alloc("ss", [128, NT], F32)
        rstd = A.alloc("rstd", [128, NT], F32)
        wr = A.alloc("wr", [128, 8, 36], F32)
        LDs(wr[:, :, 0:4], w_rg[l].rearrange("(k p) g -> p k g", p=128), [], [('wr', 0)])
        LDs(wr[:, :, 4:36], w_re[l].rearrange("(k p) g -> p k g", p=128), [], [('wr', 1)])
        V(_I('memset', ss[:], 0.0), [], ['ss'])
        for t in range(NT):
            x_ = xt[t % 2]
            LD(x_[:], out[t * 128:(t + 1) * 128, :], [('xres', t)], [('xt', t % 2)])
            Ac(_I('activation', out=junk[:], in_=x_[:], func=AF.Square, accum_out=ss[:, t:t + 1]),
               [('xt', t % 2), 'ss'], ['junk', ('ss', t)])
            rstd_from_ss(ss[:, t:t + 1], rstd[:, t:t + 1], 1.0 / D, [('ss', t)], [('rstd', t)])
            V(_I('scalar_tensor_tensor', out=htmp[:], in0=x_[:], scalar=rstd[:, t:t + 1], in1=rowA[:],
                                                           op0=ALU.mult, op1=ALU.mult), [('xt', t % 2), ('rstd', t), 'rowA'], ['htmp'])
            hf_ = h2f[t % 2]
            V(_I('tensor_tensor', out=hf_[:], in0=htmp[:], in1=rowB[:], op=ALU.add),
              ['htmp', 'rowB'], [('h2f', t % 2)])
            Ac(_I('activation', out=h2tm[:, t, :], in_=hf_[:], func=AF.Copy), [('h2f', t % 2)], [('h2tm', t)])
            for k in range(8):
                TR(psB[:, k * 128:(k + 1) * 128], hf_[:, k * 128:(k + 1) * 128], ident_f[:],
                   [('h2f', t % 2), 'ident_f'], [('psB', 'tr')])
            hT_ = h2T[t % 2]
            V(_I('tensor_copy', out=hT_[:], in_=psB[:].rearrange("p (k m) -> p k m", k=8)),
              [('psB', 'tr')], [('h2T', t % 2)])
            pl = psA[:, (t % 2) * 512:(t % 2) * 512 + 36]
            for k in range(8):
                MM(pl, hT_[:, k, :], wr[:, k, :], k == 0, k == 7, [('h2T', t % 2), ('wr', 0), ('wr', 1)], [('psA', 'lg', t % 2)])
            Ac(_I('activation', out=lgr[:, t, :], in_=pl, func=AF.Copy), [('psA', 'lg', t % 2)], [('lgr', t)])
        P.barrier()
        A.release(mk)
        if 'lgr' in taps and l == 0:
            LD(tap_out('lgr', [128, NT, 36]), lgr[:], [], ['tap_lgr'])

        if l == 0:
            stop_if('lgr')
        mk = A.mark()
        rb = A.alloc("rb", [128, 36], F32)
        LD(rb[:, 0:4], b_rg[l].partition_broadcast(128), [], [('rb', 0)])
        LD(rb[:, 4:36], b_re[l].partition_broadcast(128), [], [('rb', 1)])
        lg = A.alloc("lg", [128, NT, 36], F32)
        R = ['r']
        V(_I('tensor_tensor', out=lg[:], in0=lgr[:], in1=rb[:, :].unsqueeze(1).to_broadcast([128, NT, 36]), op=ALU.add),
          [('rb', 0), ('rb', 1)], R)
        gmax = A.alloc("gmax", [128, NT], F32)
        gsh = A.alloc("gsh", [128, NT, 4], F32)
        gex = A.alloc("gex", [128, NT, 4], F32)
        gw = A.alloc("gw", [128, NT], F32)
        pen = A.alloc("pen", [128, NT, 4], F32)
        em = A.alloc("em", [128, NT, 32], F32)
        em2 = A.alloc("em2", [128, NT, 32], F32)
        m1 = A.alloc("m1", [128, NT], F32)
        m2 = A.alloc("m2", [128, NT], F32)
        oh = [A.alloc("oh%d" % k, [128, NT, 32], F32) for k in range(2)]
        ohb = A.alloc("ohb", [128, NT * 32], BF16)
        V(_I('tensor_reduce', out=gmax[:], in_=lg[:, :, 0:4], axis=AX.X, op=ALU.max), R, R)
        V(_I('tensor_tensor', out=gsh[:], in0=lg[:, :, 0:4], in1=gmax[:, :].unsqueeze(2).to_broadcast([128, NT, 4]),
                                    op=ALU.subtract), R, R)
        Ac(_I('activation', out=gex[:], in_=gsh[:], func=AF.Exp), R, R)
        V(_I('tensor_reduce', out=gw[:], in_=gex[:], axis=AX.X, op=ALU.add), R, R)
        V(_I('reciprocal', out=gw[:], in_=gw[:]), R, R)
        V(_I('tensor_single_scalar', out=pen[:], in_=gsh[:], scalar=0.0, op=ALU.is_equal), R, R)
        V(_I('tensor_scalar', out=pen[:], in0=pen[:], scalar1=-1.0, scalar2=1e30, op0=ALU.add, op1=ALU.mult), R, R)
        V(_I('tensor_tensor', out=em[:].rearrange("p t (g x) -> p t g x", x=8),
                                    in0=lg[:, :, 4:36].rearrange("p t (g x) -> p t g x", x=8),
                                    in1=pen[:, :, :].unsqueeze(3).to_broadcast([128, NT, 4, 8]), op=ALU.add), R, R)
        V(_I('tensor_reduce', out=m1[:], in_=em[:], axis=AX.X, op=ALU.max), R, R)
        V(_I('tensor_tensor', out=oh[0][:], in0=em[:], in1=m1[:, :].unsqueeze(2).to_broadcast([128, NT, 32]),
                                    op=ALU.is_equal), R, R)
        V(_I('scalar_tensor_tensor', out=em2[:], in0=oh[0][:], scalar=-1e30, in1=em[:], op0=ALU.mult, op1=ALU.add), R, R)
        V(_I('tensor_reduce', out=m2[:], in_=em2[:], axis=AX.X, op=ALU.max), R, R)
        V(_I('tensor_tensor', out=oh[1][:], in0=em2[:], in1=m2[:, :].unsqueeze(2).to_broadcast([128, NT, 32]),
                                    op=ALU.is_equal), R, R)
        V(_I('tensor_tensor', out=m2[:], in0=m2[:], in1=m1[:], op=ALU.subtract), R, R)
        Ac(_I('activation', out=m2[:], in_=m2[:], func=AF.Exp), R, R)
        V(_I('tensor_scalar_add', out=m2[:], in0=m2[:], scalar1=1.0), R, R)
        V(_I('reciprocal', out=m2[:], in_=m2[:]), R, R)
        V(_I('tensor_tensor', out=wk[0][:], in0=m2[:], in1=gw[:], op=ALU.mult), R, R)
        V(_I('tensor_tensor', out=wk[1][:], in0=gw[:], in1=wk[0][:], op=ALU.subtract), R, R)
        V(_I('tensor_tensor', out=ohb[:], in0=oh[0][:].rearrange("p t x -> p (t x)"),
                                    in1=oh[1][:].rearrange("p t x -> p (t x)"), op=ALU.add), R, R)
        MM(psA[:, 0:512], ustrict[:], ohb[:], True, True, R + ['ustrict'], [('psA', 'pre')])
        MM(psA[:, 512:1024], ones1[:], ohb[:], True, True, R + ['ones1'], [('psA', 'tot')])
        scan_mask = A.alloc("scan_mask", [128, NE, NT], F32)
        V(_I('memset', scan_mask[:], 1.0), [], ['scan_mask'])
        V(_I('memset', scan_mask[:, :, 0:1], 0.0), ['scan_mask'], ['scan_mask'])
        tot = A.alloc("tot", [128, NE, NT], F32)
        cumi = A.alloc("cumi", [128, NE, NT], F32)
        Ac(_I('activation', out=tot[:], in_=psA[:, 512:1024].rearrange("p (t x) -> p x t", x=32), func=AF.Copy),
           [('psA', 'tot')], R)
        V(_I('tensor_tensor_scan', out=cumi[:].rearrange("p x t -> p (x t)"), data0=scan_mask[:].rearrange("p x t -> p (x t)"),
                                         data1=tot[:].rearrange("p x t -> p (x t)"), initial=0.0, op0=ALU.mult, op1=ALU.add),
          R + ['scan_mask'], R)
        npad = A.alloc("npad", [128, NE], F32)
        ntmp = A.alloc("ntmp", [128, NE], F32)
        pend = A.alloc("pend", [128, NE], F32)
        cmpn = A.alloc("cmpn", [128, NE, NT], F32)
        V(_I('tensor_tensor', out=cmpn[:], in0=cumi[:, :, NT - 1:NT].to_broadcast([128, NE, NT]),
                                    in1=b128[:, 0:NT].unsqueeze(1).to_broadcast([128, NE, NT]), op=ALU.is_gt), R + ['b128'], R)
        V(_I('tensor_reduce', out=npad[:], in_=cmpn[:], axis=AX.X, op=ALU.add), R, R)
        V(_I('tensor_scalar_mul', out=npad[:], in0=npad[:], scalar1=128.0), R, R)
        V(_I('tensor_tensor_scan', out=pend[:], data0=ones_row[:], data1=npad[:], initial=0.0, op0=ALU.mult, op1=ALU.add),
          R + ['ones_row'], R)
        V(_I('tensor_tensor', out=cumi[:], in0=cumi[:], in1=tot[:], op=ALU.subtract), R, R)
        V(_I('tensor_tensor', out=ntmp[:], in0=pend[:], in1=npad[:], op=ALU.subtract), R, R)
        V(_I('tensor_tensor', out=cumi[:], in0=cumi[:], in1=ntmp[:, :].unsqueeze(2).to_broadcast([128, NE, NT]), op=ALU.add), R, R)
        dest = A.alloc("dest", [128, NT, NE], F32)
        V(_I('tensor_tensor', out=dest[:], in0=psA[:, 0:512].rearrange("p (t x) -> p t x", x=32),
                                    in1=cumi[:].rearrange("p x t -> p t x"), op=ALU.add), R + [('psA', 'pre')], R)
        dsel = A.alloc("dsel", [128, NT, NE], F32)
        dfl = A.alloc("dfl", [128, NT], F32)
        for k in range(2):
            V(_I('tensor_tensor', out=dsel[:], in0=dest[:], in1=oh[k][:], op=ALU.mult), R, R)
            V(_I('tensor_reduce', out=dfl[:], in_=dsel[:], axis=AX.X, op=ALU.add), R, R)
            V(_I('tensor_copy', out=d_i[k][:], in_=dfl[:]), R, [('d_i', k)])
        cmp_ = A.alloc("cmp_", [128, NBLK, NE], F32)
        bef = A.alloc("bef", [128, NBLK], F32)
        V(_I('tensor_tensor', out=cmp_[:], in0=pend[:, :].unsqueeze(1).to_broadcast([128, NBLK, NE]),
                                    in1=b128[:, :].unsqueeze(2).to_broadcast([128, NBLK, NE]), op=ALU.is_le), R + ['b128'], R)
        V(_I('tensor_reduce', out=bef[:], in_=cmp_[:], axis=AX.X, op=ALU.add), R, R)
        V(_I('tensor_scalar_min', out=bef[:], in0=bef[:], scalar1=float(NE - 1)), R, R)
        basep = A.alloc("basep", [128, 1], F32)
        wif = A.alloc("wif", [128, NBLK], F32)
        G(_I('iota', basep[:], pattern=[[0, 1]], base=l * NE * 128, channel_multiplier=1,
             allow_small_or_imprecise_dtypes=True), [], ['basep'])
        V(_I('tensor_scalar', out=wif[:], in0=bef[:], scalar1=128.0, scalar2=basep[:, 0:1], op0=ALU.mult, op1=ALU.add),
          R + ['basep'], R)
        V(_I('tensor_copy', out=widx[:], in_=wif[:]), R, ['widx'])
        base2 = A.alloc("base2", [128, 4], F32)
        wif2 = A.alloc("wif2", [128, NBLK, 4], F32)
        G(_I('iota', base2[:], pattern=[[128, 4]], base=l * NE * W, channel_multiplier=1,
             allow_small_or_imprecise_dtypes=True), [], ['base2'])
        V(_I('scalar_tensor_tensor', out=wif2[:], in0=bef[:, :].unsqueeze(2).to_broadcast([128, NBLK, 4]), scalar=float(W),
             in1=base2[:, :].unsqueeze(1).to_broadcast([128, NBLK, 4]), op0=ALU.mult, op1=ALU.add), R + ['base2'], R)
        V(_I('tensor_copy', out=widx2[:], in_=wif2[:]), R, ['widx2'])
        if 'route' in taps and l == 0:
            LD(tap_out('d_i0', [128, NT], I32), d_i[0][:], [('d_i', 0)], ['tap_d0'])
            LD(tap_out('d_i1', [128, NT], I32), d_i[1][:], [('d_i', 1)], ['tap_d1'])
            LD(tap_out('wk0', [128, NT]), wk[0][:], R, ['tap_w0'])
            LD(tap_out('wk1', [128, NT]), wk[1][:], R, ['tap_w1'])
            LD(tap_out('widx', [128, NBLK], I32), widx[:], ['widx'], ['tap_be'])
        if l == 0:
            stop_if('route')
        P.barrier()
        A.release(mk)

        for t in range(NT):
            for k in range(2):
                P.dma('pool', _I('indirect_dma_start',
                    out=xb_d, out_offset=bass.IndirectOffsetOnAxis(ap=d_i[k][:, t:t + 1], axis=0),
                    in_=h2tm[:, t, :], in_offset=None), [], [('xb_d', t, k)])
        P.barrier()

        A.release(moe_mark)
        mk = A.mark()
        w1s = [A.alloc("w1s%d" % i, [128, 8, W], F32) for i in range(2)]
        w3s = [A.alloc("w3s%d" % i, [128, 8, W], F32) for i in range(2)]
        w2s = [A.alloc("w2s%d" % i, [128, 4, D], F32) for i in range(2)]
        w1b = [A.alloc("w1b%d" % i, [128, 8, W], BF16) for i in range(2)]
        w3b = [A.alloc("w3b%d" % i, [128, 8, W], BF16) for i in range(2)]
        w2b = [A.alloc("w2b%d" % i, [128, 4, D], BF16) for i in range(2)]
        xbt = [A.alloc("xbt%d" % i, [128, D], BF16) for i in range(2)]
        xbT = [A.alloc("xbT%d" % i, [128, 8, 128], BF16) for i in range(2)]
        sgh = [A.alloc("sgh%d" % i, [128, 512], F32) for i in range(2)]
        hidT = [A.alloc("hidT%d" % i, [128, 4, 128], BF16) for i in range(2)]
        ybs = [A.alloc("ybs%d" % i, [128, D], F32) for i in range(2)]
        eg_rows = w_eg.rearrange("l e (p j) f -> (l e p) (j f)", j=8)
        eu_rows = w_eu.rearrange("l e (p j) f -> (l e p) (j f)", j=8)
        ed_rows = w_ed.rearrange("l e r f -> (l e r) f")

        def issue_loads(b):
            wi = b % 2
            for (dst, rows_, key) in ((w1s, eg_rows, 'w1s'), (w3s, eu_rows, 'w3s')):
                P.dma('pool', _I('indirect_dma_start', out=dst[wi][:].rearrange("p k f -> p (k f)"), out_offset=None, in_=rows_,
                                 in_offset=bass.IndirectOffsetOnAxis(ap=widx[:, b:b + 1], axis=0)), [], [(key, wi)])
            for k in range(4):
                P.dma('pool', _I('indirect_dma_start', out=w2s[wi][:, k, :], out_offset=None, in_=ed_rows,
                                 in_offset=bass.IndirectOffsetOnAxis(ap=widx2[:, b, k:k + 1], axis=0)), [], [('w2s', wi, k)])
            LD(xbt[wi][:], xb_d[b * 128:(b + 1) * 128, :], [], [('xbt', wi)])
        issue_loads(0)
        for b in range(NBLK):
            i_ = b % 2
            wi = b % 2
            if b + 1 < NBLK:
                issue_loads(b + 1)
            V(_I('tensor_copy', out=w1b[wi][:], in_=w1s[wi][:]), [('w1s', wi)], [('w1b', wi)])
            V(_I('tensor_copy', out=w3b[wi][:], in_=w3s[wi][:]), [('w3s', wi)], [('w3b', wi)])
            V(_I('tensor_copy', out=w2b[wi][:], in_=w2s[wi][:]), [('w2s', wi, k) for k in range(4)], [('w2b', wi)])
            xv = xbt[i_][:].rearrange("s (p j) -> s j p", j=8)
            for k in range(8):
                TR(psT[:, i_, k * 128:(k + 1) * 128], xv[:, k, :], ident_b[:], [('xbt', i_)], [('psT', i_)])
            V(_I('tensor_copy', out=xbT[i_][:], in_=psT[:, i_, :].rearrange("p (k m) -> p k m", k=8)),
              [('psT', i_)], [('xbT', i_)])
            for (wt, wkey, off) in ((w1b[wi], ('w1b', wi), 0), (w3b[wi], ('w3b', wi), 512)):
                for fc in range(4):
                    for k in range(8):
                        MM(psA[:, i_ * 1024 + off + fc * 128: i_ * 1024 + off + (fc + 1) * 128], wt[:, k, fc * 128:(fc + 1) * 128],
                           xbT[i_][:, k, :], k == 0, k == 7, [wkey, ('xbT', i_)], [('psA', 'gu', i_, off)])
            Ac(_I('activation', out=sgh[i_][:], in_=psA[:, i_ * 1024:i_ * 1024 + 512], func=AF.Silu),
               [('psA', 'gu', i_, 0)], [('sgh', i_)])
            V(_I('tensor_tensor', out=hidT[i_][:].rearrange("p f m -> p (f m)"), in0=psA[:, i_ * 1024 + 512:i_ * 1024 + 1024],
                                               in1=sgh[i_][:], op=ALU.mult), [('psA', 'gu', i_, 512), ('sgh', i_)], [('hidT', i_)])
            for hf in range(2):
                for fc in range(4):
                    MM(psB[:, hf * 512:(hf + 1) * 512], hidT[i_][:, fc, :], w2b[wi][:, fc, hf * 512:(hf + 1) * 512], fc == 0, fc == 3,
                       [('hidT', i_), ('w2b', wi)], [('psB', 'yb')])
            Ac(_I('activation', out=ybs[i_][:], in_=psB[:], func=AF.Copy), [('psB', 'yb')], [('ybs', i_)])
            LD(yb_d[b * 128:(b + 1) * 128, :], ybs[i_][:], [('ybs', i_)], [('yb_d', b)])
        P.barrier()
        A.release(mk)

        mk = A.mark()
        rowG = load_row(5)
        g1t = [A.alloc("g1t%d" % i, [128, D], F32) for i in range(2)]
        g2t = [A.alloc("g2t%d" % i, [128, D], F32) for i in range(2)]
        xt = [A.alloc("xt%d" % i, [128, D], F32) for i in range(2)]
        last = (l == n_layers - 1) and do_final
        if last:
            fg = A.alloc("fg", [128, D], F32)
            LD(fg[:], fin_g.partition_broadcast(128), [], ['fg'])
            ss = A.alloc("ss", [128, NT], F32)
            rstd = A.alloc("rstd", [128, NT], F32)
            junk = A.alloc("junk", [128, D], BF16)
            V(_I('memset', ss[:], 0.0), [], ['ss'])
        for t in range(NT):
            i_ = t % 2
            P.dma('pool', _I('indirect_dma_start',
                out=g1t[i_][:], out_offset=None, in_=yb_d,
                in_offset=bass.IndirectOffsetOnAxis(ap=d_i[0][:, t:t + 1], axis=0)), [], [('g1t', i_)])
            P.dma('pool', _I('indirect_dma_start',
                out=g2t[i_][:], out_offset=None, in_=yb_d,
                in_offset=bass.IndirectOffsetOnAxis(ap=d_i[1][:, t:t + 1], axis=0)), [], [('g2t', i_)])
            LD(xt[i_][:], out[t * 128:(t + 1) * 128, :], [('xres', t)], [('xt', i_)])
            V(_I('tensor_scalar_mul', out=g1t[i_][:], in0=g1t[i_][:], scalar1=wk[0][:, t:t + 1]), [('g1t', i_)], [('g1t', i_)])
            V(_I('scalar_tensor_tensor', out=g1t[i_][:], in0=g2t[i_][:], scalar=wk[1][:, t:t + 1], in1=g1t[i_][:],
                                                           op0=ALU.mult, op1=ALU.add), [('g1t', i_), ('g2t', i_)], [('g1t', i_)])
            V(_I('tensor_tensor', out=g1t[i_][:], in0=g1t[i_][:], in1=rowG[:], op=ALU.mult), [('g1t', i_), 'rowG'], [('g1t', i_)])
            V(_I('tensor_tensor', out=xt[i_][:], in0=xt[i_][:], in1=g1t[i_][:], op=ALU.add), [('g1t', i_), ('xt', i_)], [('xt', i_)])
            if last:
                Ac(_I('activation', out=junk[:], in_=xt[i_][:], func=AF.Square, accum_out=ss[:, t:t + 1]),
                   [('xt', i_), 'ss'], ['junk', ('ss', t)])
                rstd_from_ss(ss[:, t:t + 1], rstd[:, t:t + 1], 1.0 / D, [('ss', t)], [('rstd', t)])
                V(_I('scalar_tensor_tensor', out=xt[i_][:], in0=xt[i_][:], scalar=rstd[:, t:t + 1], in1=fg[:],
                                                               op0=ALU.mult, op1=ALU.mult), [('xt', i_), ('rstd', t), 'fg'], [('xt', i_)])
            LD(out[t * 128:(t + 1) * 128, :], xt[i_][:], [('xt', i_)], [('xres', t)])
        P.barrier()
        A.release(mk)

    P.barrier()
    P.emit(st)
    st.close()
    print("SBUF peak bytes", A.peak, "limit", A.limit)
    return nc, list(tap_aps.keys())


INPUT_NAMES = ['x', 'c', 'norm_mix_g', 'norm_ffn_g', 'w_ada', 'b_ada', 'w_in', 'lam_re', 'lam_im', 'log_step',
               'ssm_b_re', 'ssm_b_im', 'ssm_c_re', 'ssm_c_im', 'ssm_d', 'w_glu', 'conf_dw_w', 'conf_dw_b',
               'conf_ln_g', 'conf_ln_b', 'sconv_w', 'w_branch', 'w_gate', 'b_gate', 'w_out', 'w_router_group',
               'b_router_group', 'w_router_expert', 'b_router_expert', 'w_exp_gate', 'w_exp_up', 'w_exp_down',
               'final_norm_g']

_CACHE = {}


def kernel(**inputs):
    if 'nc' not in _CACHE:
        _CACHE['nc'] = build_program()[0]
    nc = _CACHE['nc']
    arrs = {k: np.ascontiguousarray(np.asarray(inputs[k], dtype=np.float32)) for k in INPUT_NAMES}
    in_maps = []
    for b in range(8):
        m = {k: v for k, v in arrs.items() if k not in ('x', 'c')}
        m['x'] = np.ascontiguousarray(arrs['x'][b])
        m['c'] = np.ascontiguousarray(arrs['c'][b])
        in_maps.append(m)
    res = run_bass_kernel_spmd(nc, in_maps, core_ids=list(range(8)))
    return np.stack([np.asarray(r["out"], dtype=np.float32) for r in res.results], axis=0)
```

```python
import math
from contextlib import ExitStack
import numpy as np
import concourse.bass as bass
import concourse.mybir as mybir
from concourse.bass_utils import run_bass_kernel_spmd

F32 = mybir.dt.float32
BF16 = mybir.dt.bfloat16
I32 = mybir.dt.int32
ALU = mybir.AluOpType
AF = mybir.ActivationFunctionType
AX = mybir.AxisListType

D = 1024
S = 2048
W = 512
NL = 4
NT = 16
NE = 32
NBLK = 64
CAP = NBLK * 128
EPS = 1e-6
TWO_PI = 2.0 * math.pi

ENGS = ['pe', 'act', 'dve', 'pool', 'sp']
SAME_ENG_SYNC = True


class Prog:
    def __init__(self, nc, ndma=8):
        self.nc = nc
        self.ops = {e: [] for e in ENGS}
        self.cnt = {e: 0 for e in ENGS}
        self.seen = {e: {} for e in ENGS}
        self.res = {}
        self.ndma = ndma
        self.dma_i = {e: 0 for e in ENGS}
        self.dma_exp = {}
        self.semkeys = set()

    def _need(self, eng, dep, waits):
        if dep is None:
            return
        key, val = dep
        if key == ('E', eng) and (eng == 'pe' or not SAME_ENG_SYNC):
            return
        if self.seen[eng].get(key, 0) >= val:
            return
        if waits.get(key, 0) < val:
            waits[key] = val

    def _deps(self, eng, reads, writes, waits):
        for r in reads:
            st = self.res.get(r)
            if st:
                self._need(eng, st[0], waits)
        for w in writes:
            st = self.res.get(w)
            if st:
                self._need(eng, st[0], waits)
                for d in st[1]:
                    self._need(eng, d, waits)

    def _commit(self, eng, mydep, reads, writes, waits):
        for key, val in waits.items():
            self.seen[eng][key] = val
        for r in reads:
            self.res.setdefault(r, [None, []])[1].append(mydep)
        for w in writes:
            self.res[w] = [mydep, []]

    def op(self, eng, fn, reads=(), writes=()):
        waits = {}
        self._deps(eng, reads, writes, waits)
        self.cnt[eng] += 1
        key = ('E', eng)
        self.semkeys.add(key)
        mydep = (key, self.cnt[eng])
        self.ops[eng].append((list(waits.items()), fn, key, 1))
        self._commit(eng, mydep, reads, writes, waits)
        return mydep

    def dma(self, q, fn, reads=(), writes=()):
        i = self.dma_i[q] % self.ndma
        self.dma_i[q] += 1
        skey = ('D', q, i)
        self.semkeys.add(skey)
        prev = self.dma_exp.get(skey, 0)
        waits = {}
        if prev > 0:
            self._need(q, (skey, prev), waits)
        self._deps(q, reads, writes, waits)
        self.dma_exp[skey] = prev + 16
        mydep = (skey, prev + 16)
        self.ops[q].append((list(waits.items()), fn, skey, 16))
        self._commit(q, mydep, reads, writes, waits)
        return mydep

    def barrier(self):
        deps = [(('E', e), self.cnt[e]) for e in ENGS if self.cnt[e] > 0]
        deps += [(k, v) for k, v in self.dma_exp.items()]
        for e in ENGS:
            waits = {}
            for d in deps:
                self._need(e, d, waits)
            if waits:
                self.ops[e].append((list(waits.items()), None, None, 0))
                for key, val in waits.items():
                    self.seen[e][key] = val
        self.res = {}

    def emit(self, stack):
        nc = self.nc
        sems = {}
        for k in sorted(self.semkeys, key=str):
            sems[k] = stack.enter_context(nc.semaphore("s_" + "_".join(str(x) for x in k)))
        block = stack.enter_context(nc.Block())
        battr = {'pe': 'tensor', 'act': 'scalar', 'dve': 'vector', 'pool': 'gpsimd', 'sp': 'sync'}

        def mk(e):
            def body(eng):
                for waits, fn, key, inc in self.ops[e]:
                    for wk, wv in waits:
                        eng.wait_ge(sems[wk], wv)
                    if fn is not None:
                        ins = fn(eng)
                        ins.then_inc(sems[key], inc)
            return body
        for e in ENGS:
            if self.ops[e]:
                getattr(block, battr[e])(mk(e))


def _I(name, *args, **kw):
    return lambda e: getattr(e, name)(*args, **kw)


class Arena:
    def __init__(self, nc, start=16576, limit=229344 - 64):
        self.nc, self.off, self.limit, self.n = nc, start, limit, 0

    def alloc(self, name, shape, dt):
        sz = {F32: 4, BF16: 2, I32: 4}[dt]
        nb = sz
        for s in shape[1:]:
            nb *= s
        nb = (nb + 31) // 32 * 32
        self.n += 1
        t = self.nc.alloc_sbuf_tensor_at("%s_%d" % (name, self.n), list(shape), dt, offset=self.off)
        self.off += nb
        self.peak = max(getattr(self, 'peak', 0), self.off)
        assert self.off <= self.limit, ("SBUF overflow", name, self.off)
        return t

    def mark(self):
        return self.off

    def release(self, m):
        self.off = m


class _Stop(Exception):
    pass


def build_program(n_layers=NL, taps=(), do_final=True):
    nc = bass.Bass("TRN2", target_bir_lowering=False)
    try:
        return _build_body(nc, n_layers, taps, do_final)
    except _Stop as e:
        return e.args[0]


def _build_body(nc, n_layers, taps, do_final):

    def din(name, shape, dt=F32):
        return nc.dram_tensor(name, list(shape), dt, kind="ExternalInput").ap()
    x_in = din("x", [S, D])
    c_in = din("c", [D])
    norm_mix_g = din("norm_mix_g", [NL, D])
    norm_ffn_g = din("norm_ffn_g", [NL, D])
    w_ada = din("w_ada", [NL, D, 6 * D])
    b_ada = din("b_ada", [NL, 6 * D])
    w_in = din("w_in", [NL, D, 6 * W])
    lam_re = din("lam_re", [NL, 32, 64])
    lam_im = din("lam_im", [NL, 32, 64])
    log_step = din("log_step", [NL, 32])
    ssm_b_re = din("ssm_b_re", [NL, 32, 64, 16])
    ssm_b_im = din("ssm_b_im", [NL, 32, 64, 16])
    ssm_c_re = din("ssm_c_re", [NL, 32, 16, 64])
    ssm_c_im = din("ssm_c_im", [NL, 32, 16, 64])
    ssm_d = din("ssm_d", [NL, W])
    w_glu = din("w_glu", [NL, W, W])
    conf_dw_w = din("conf_dw_w", [NL, 31, W])
    conf_dw_b = din("conf_dw_b", [NL, W])
    conf_ln_g = din("conf_ln_g", [NL, W])
    conf_ln_b = din("conf_ln_b", [NL, W])
    sconv_w = din("sconv_w", [NL, 3, W])
    w_branch = din("w_branch", [NL, 3, W, D])
    w_gate = din("w_gate", [NL, D, 3 * D])
    b_gate = din("b_gate", [NL, 3 * D])
    w_out = din("w_out", [NL, D, D])
    w_rg = din("w_router_group", [NL, D, 4])
    b_rg = din("b_router_group", [NL, 4])
    w_re = din("w_router_expert", [NL, D, NE])
    b_re = din("b_router_expert", [NL, NE])
    w_eg = din("w_exp_gate", [NL, NE, D, W])
    w_eu = din("w_exp_up", [NL, NE, D, W])
    w_ed = din("w_exp_down", [NL, NE, W, D])
    fin_g = din("final_norm_g", [D])
    out = nc.dram_tensor("out", [S, D], F32, kind="ExternalOutput").ap()
    mod_d = nc.dram_tensor("mod_d", [6 * D], F32, kind="Internal").ap()
    xb_d = nc.dram_tensor("xb_d", [CAP, D], BF16, kind="Internal").ap()
    yb_d = nc.dram_tensor("yb_d", [CAP, D], F32, kind="Internal").ap()
    tap_aps = {}

    def tap_out(name, shape, dt=F32):
        tap_aps[name] = nc.dram_tensor("tap_" + name, list(shape), dt, kind="ExternalOutput").ap()
        return tap_aps[name]

    st = ExitStack()
    P = Prog(nc)

    def stop_if(name):
        if ('stop:' + name) in taps:
            P.barrier()
            P.emit(st)
            st.close()
            raise _Stop((nc, list(tap_aps.keys())))
    A = Arena(nc)
    psA = st.enter_context(nc.psum_tensor("psA", [128, 2048], F32))
    psB = st.enter_context(nc.psum_tensor("psB", [128, 1024], F32))
    psT = st.enter_context(nc.psum_tensor("psT", [128, 2, 1024], BF16))

    def V(fn, reads, writes):
        return P.op('dve', fn, reads, writes)

    def Ac(fn, reads, writes):
        return P.op('act', fn, reads, writes)

    def G(fn, reads, writes):
        return P.op('pool', fn, reads, writes)

    def MM(o, lhsT, rhs, start, stop, reads, writes):
        return P.op('pe', _I('matmul', o, lhsT=lhsT, rhs=rhs, start=start, stop=stop), reads, writes)

    def TR(o, in_, ident, reads, writes):
        return P.op('pe', _I('transpose', o, in_, ident), reads, writes)

    def LD(o, i, reads=(), writes=(), q='sp'):
        return P.dma(q, _I('dma_start', out=o, in_=i), reads, writes)

    def LDs(o, i, reads=(), writes=(), q='sp'):
        return P.dma(q, _I('dma_start', out=o, in_=i, allow_slow_non_contiguous=True), reads, writes)

    ident_f = A.alloc("ident_f", [128, 128], F32)
    ident_b = A.alloc("ident_b", [128, 128], BF16)
    ones_d = A.alloc("ones_d", [128, 128], BF16)
    ones1 = A.alloc("ones1", [128, 128], BF16)
    ustrict = A.alloc("ustrict", [128, 128], BF16)
    iota_t = A.alloc("iota_t", [128, S], F32)
    eps_t = A.alloc("eps_t", [128, 1], F32)
    pi_t = A.alloc("pi_t", [128, 1], F32)
    hpi_t = A.alloc("hpi_t", [128, 1], F32)
    condb = A.alloc("condb", [128, 8], BF16)
    b128 = A.alloc("b128", [128, NBLK], F32)
    ones_row = A.alloc("ones_row", [128, NE], F32)
    iota_e = A.alloc("iota_e", [128, NE], F32)

    G(_I('memset', ident_f[:], 0.0), [], ['ident_f'])
    G(_I('affine_select', out=ident_f[:], in_=ident_f[:], pattern=[[-1, 128]], compare_op=ALU.not_equal,
                                fill=1.0, base=0, channel_multiplier=1), [], ['ident_f'])
    V(_I('tensor_copy', out=ident_b[:], in_=ident_f[:]), ['ident_f'], ['ident_b'])
    V(_I('memset', ones_d[:], 1.0 / 512.0), [], ['ones_d'])
    V(_I('memset', ones1[:], 1.0), [], ['ones1'])
    G(_I('memset', ustrict[:], 1.0), [], ['ustrict'])
    G(_I('affine_select', out=ustrict[:], in_=ustrict[:], pattern=[[1, 128]], compare_op=ALU.is_gt,
                                fill=0.0, base=0, channel_multiplier=-1), [], ['ustrict'])
    G(_I('iota', iota_t[:], pattern=[[1, S]], base=0, channel_multiplier=0,
                       allow_small_or_imprecise_dtypes=True), [], ['iota_t'])
    G(_I('iota', b128[:], pattern=[[128, NBLK]], base=0, channel_multiplier=0,
                       allow_small_or_imprecise_dtypes=True), [], ['b128'])
    G(_I('iota', iota_e[:], pattern=[[1, NE]], base=0, channel_multiplier=0,
                       allow_small_or_imprecise_dtypes=True), [], ['iota_e'])
    V(_I('memset', eps_t[:], EPS), [], ['eps_t'])
    V(_I('memset', pi_t[:], math.pi), [], ['pi_t'])
    V(_I('memset', hpi_t[:], math.pi / 2), [], ['hpi_t'])
    V(_I('memset', ones_row[:], 1.0), [], ['ones_row'])
    m0 = A.mark()
    c_sb = A.alloc("c_sb", [128, 8], F32)
    LDs(c_sb[:], c_in.rearrange("(k p) -> p k", p=128), [], ['c_sb'])
    Ac(_I('activation', out=condb[:], in_=c_sb[:], func=AF.Silu), ['c_sb'], ['condb'])
    P.barrier()
    A.release(m0)
    base_mark = A.mark()

    def rstd_from_ss(ss_ap, rstd_ap, scale, keys_r, keys_w):
        Ac(_I('activation', out=rstd_ap, in_=ss_ap, func=AF.Sqrt, bias=eps_t[:, 0:1], scale=scale), keys_r, keys_w)
        V(_I('reciprocal', out=rstd_ap, in_=rstd_ap), keys_w, keys_w)

    INV2PI = 1.0 / TWO_PI

    MAGIC = 12582912.0

    def range_reduce(x_ap, n_ap, m_ap, rk, wk_):
        V(_I('tensor_scalar', out=m_ap, in0=x_ap, scalar1=INV2PI, scalar2=MAGIC, op0=ALU.mult, op1=ALU.add), rk, wk_)
        V(_I('tensor_scalar', out=m_ap, in0=m_ap, scalar1=-MAGIC, scalar2=-TWO_PI, op0=ALU.add, op1=ALU.mult), wk_, wk_)
        V(_I('tensor_tensor', out=x_ap, in0=x_ap, in1=m_ap, op=ALU.add), wk_, wk_)

    for l in range(n_layers):
        xsrc = x_in if l == 0 else out
        A.release(base_mark)
        mk = A.mark()
        barow = A.alloc("barow", [1, 6 * D], F32)
        LD(barow[0:1, :], b_ada[l:l + 1, :], [], ['barow'])
        wadab = [A.alloc("wada%d" % i, [128, 8, 512], BF16) for i in range(2)]
        mtmp = [A.alloc("mtmp%d" % i, [1, 512], F32) for i in range(2)]
        for n in range(12):
            wb = wadab[n % 2]
            LD(wb[:], w_ada[l, :, n * 512:(n + 1) * 512].rearrange("(k p) f -> p k f", p=128), [], [('wada', n % 2)], q='pool')
            for k in range(8):
                MM(psA[0:1, (n % 2) * 512:(n % 2) * 512 + 512], condb[:, k:k + 1], wb[:, k, :], k == 0, k == 7,
                   ['condb', ('wada', n % 2)], ['pA%d' % (n % 2)])
            mt = mtmp[n % 2]
            V(_I('tensor_tensor', out=mt[0:1, :], in0=psA[0:1, (n % 2) * 512:(n % 2) * 512 + 512],
                                                    in1=barow[0:1, n * 512:(n + 1) * 512], op=ALU.add),
              ['pA%d' % (n % 2), 'barow'], [('mtmp', n % 2)])
            LD(mod_d[n * 512:(n + 1) * 512].rearrange("(o f) -> o f", o=1), mt[0:1, :], [('mtmp', n % 2)], [('mod_d', n)])
        P.barrier()
        A.release(mk)

        def load_rows(jA, jB, gsrc):
            rowA = A.alloc("rowA", [128, D], F32)
            rowB = A.alloc("rowB", [128, D], F32)
            gt_ = A.alloc("gt_", [128, D], F32)
            LD(rowA[:], mod_d[jA * D:(jA + 1) * D].partition_broadcast(128), [], ['rowA'])
            LD(rowB[:], mod_d[jB * D:(jB + 1) * D].partition_broadcast(128), [], ['rowB'])
            LD(gt_[:], gsrc[l].partition_broadcast(128), [], ['gt_'])
            V(_I('scalar_tensor_tensor', out=rowA[:], in0=rowA[:], scalar=1.0, in1=gt_[:], op0=ALU.add, op1=ALU.mult),
              ['rowA', 'gt_'], ['rowA'])
            return rowA, rowB

        def load_row(j):
            rowG = A.alloc("rowG", [128, D], F32)
            LD(rowG[:], mod_d[j * D:(j + 1) * D].partition_broadcast(128), [], ['rowG'])
            return rowG

        hT = A.alloc("hT", [128, 8, S], BF16)
        ys = [None, None, None]
        ys[0] = A.alloc("ys0", [128, 4, S], BF16)
        ys1_mark = A.mark()
        ys[1] = A.alloc("ys1", [128, 4, S], BF16)
        ys[2] = A.alloc("ys2", [128, 4, S], BF16)
        mixer_mark = A.mark()
        rowA, rowB = load_rows(1, 0, norm_mix_g)
        xt = [A.alloc("xt%d" % i, [128, D], F32) for i in range(2)]
        junk = A.alloc("junk", [128, D], BF16)
        htmp = A.alloc("htmp", [128, D], F32)
        hb = [A.alloc("hb%d" % i, [128, D], BF16) for i in range(2)]
        ss = A.alloc("ss", [128, NT], F32)
        rstd = A.alloc("rstd", [128, NT], F32)
        V(_I('memset', ss[:], 0.0), [], ['ss'])
        for t in range(NT):
            x_ = xt[t % 2]
            LD(x_[:], xsrc[t * 128:(t + 1) * 128, :], [('xres', t)], [('xt', t % 2)])
            Ac(_I('activation', out=junk[:], in_=x_[:], func=AF.Square, accum_out=ss[:, t:t + 1]),
               [('xt', t % 2), 'ss'], ['junk', ('ss', t)])
            rstd_from_ss(ss[:, t:t + 1], rstd[:, t:t + 1], 1.0 / D, [('ss', t)], [('rstd', t)])
            V(_I('scalar_tensor_tensor', out=htmp[:], in0=x_[:], scalar=rstd[:, t:t + 1], in1=rowA[:],
                                                           op0=ALU.mult, op1=ALU.mult), [('xt', t % 2), ('rstd', t), 'rowA'], ['htmp'])
            hb_ = hb[t % 2]
            V(_I('tensor_tensor', out=hb_[:], in0=htmp[:], in1=rowB[:], op=ALU.add),
              ['htmp', 'rowB'], [('hb', t % 2)])
            for k in range(8):
                TR(psT[:, t % 2, k * 128:(k + 1) * 128], hb_[:, k * 128:(k + 1) * 128], ident_b[:],
                   [('hb', t % 2), 'ident_b'], [('psT', t % 2)])
            Ac(_I('activation', out=hT[:, :, t * 128:(t + 1) * 128],
                                           in_=psT[:, t % 2, :].rearrange("p (k m) -> p k m", k=8), func=AF.Copy),
               [('psT', t % 2)], [('hT', t)])
        P.barrier()
        if 'n1dbg' not in taps:
            A.release(mixer_mark)
        if 'hT' in taps and l == 0:
            tp = tap_out('hT', [8, 128, S], BF16)
            LD(tp.rearrange("k p s -> p k s"), hT[:], [], ['tap_hT'])
        if 'n1dbg' in taps and l == 0:
            LD(tap_out('ss', [128, NT]), ss[:], [], ['tap_ss'])
            LD(tap_out('rstd', [128, NT]), rstd[:], [], ['tap_rstd'])
            LD(tap_out('rowA', [128, D]), rowA[:], [], ['tap_rowA'])
            LD(tap_out('rowB', [128, D]), rowB[:], [], ['tap_rowB'])
            LD(tap_out('hb1', [128, D], BF16), hb[1][:], [], ['tap_hb1'])
            LD(tap_out('modd', [6 * D]), mod_d, [], ['tap_modd'])
            P.barrier()
            P.emit(st)
            st.close()
            return nc, list(tap_aps.keys())

        def load_win(dst, j, key):
            LD(dst[:], w_in[l, :, j * 512:(j + 1) * 512].rearrange("(k p) f -> p k f", p=128), [], [key], q='pool')

        def proj_in(wt, wkey, ft, tc, ps_ap, pkey):
            for k in range(8):
                MM(ps_ap, wt[:, k, ft * 128:(ft + 1) * 128], hT[:, k, tc * 512:(tc + 1) * 512], k == 0, k == 7,
                   [wkey], [pkey])

        A.release(ys1_mark)
        uT = A.alloc("uT", [128, 4, S], BF16)
        rho = A.alloc("rho", [128, 16], F32)
        th = A.alloc("th", [128, 16], F32)
        Bpad = [A.alloc("Bpad%d" % c, [128, 16, 128], BF16) for c in range(2)]
        Cpad = [A.alloc("Cpad%d" % c, [128, 16, 128], BF16) for c in range(3)]
        Dd = A.alloc("Dd", [128, 4, 128], BF16)
        mk2 = A.mark()
        wu = A.alloc("wu", [128, 8, 512], BF16)
        load_win(wu, 0, 'wu')
        for ft in range(4):
            for tc in range(4):
                proj_in(wu, 'wu', ft, tc, psA[:, tc * 512:(tc + 1) * 512], 'pA%d' % tc)
                Ac(_I('activation', out=uT[:, ft, tc * 512:(tc + 1) * 512], in_=psA[:, tc * 512:(tc + 1) * 512],
                                                        func=AF.Copy), ['pA%d' % tc], [('uT', ft)])
        lr = A.alloc("lr", [128, 16], F32)
        li = A.alloc("li", [128, 16], F32)
        stp = A.alloc("stp", [128, 16], F32)
        LDs(lr[:], lam_re[l].rearrange("(q g) p -> (g p) q", g=2), [], ['lr'])
        LDs(li[:], lam_im[l].rearrange("(q g) p -> (g p) q", g=2), [], ['li'])
        ls2 = log_step[l].rearrange("(q g) -> g q", g=2)
        for g in range(2):
            LDs(stp[g * 64:(g + 1) * 64, :], ls2[g:g + 1, :].to_broadcast([64, 16]), [], [('stp', g)])
        kr = A.alloc("kr", [128, 16], F32)
        ki = A.alloc("ki", [128, 16], F32)
        sp_t = [A.alloc("spt%d" % i, [128, 16], F32) for i in range(6)]
        Ac(_I('activation', out=stp[:], in_=stp[:], func=AF.Exp), [('stp', 0), ('stp', 1)], ['stp'])
        V(_I('tensor_tensor', out=rho[:], in0=lr[:], in1=stp[:], op=ALU.mult), ['lr', 'stp'], ['rho'])
        Ac(_I('activation', out=rho[:], in_=rho[:], func=AF.Exp), ['rho'], ['rho'])
        V(_I('tensor_tensor', out=th[:], in0=li[:], in1=stp[:], op=ALU.mult), ['li', 'stp'], ['th'])
        ysn, ycs, sn, cs, den, tmpa = sp_t
        thn = A.alloc("thn", [128, 16], I32)
        V(_I('tensor_scalar_add', out=ysn[:], in0=th[:], scalar1=TWO_PI), ['th'], ['ysn'])
        range_reduce(ysn[:], thn[:], tmpa[:], ['ysn'], ['ysn', 'thn', 'tmpa'])
        Ac(_I('activation', out=sn[:], in_=ysn[:], func=AF.Sin), ['ysn'], ['sn'])
        Ac(_I('activation', out=ycs[:], in_=ysn[:], func=AF.Abs), ['ysn'], ['ycs'])
        Ac(_I('activation', out=cs[:], in_=ycs[:], func=AF.Sin, bias=hpi_t[:, 0:1], scale=-1.0), ['ycs'], ['cs'])
        V(_I('tensor_tensor', out=cs[:], in0=cs[:], in1=rho[:], op=ALU.mult), ['cs', 'rho'], ['cs'])
        V(_I('tensor_scalar_add', out=cs[:], in0=cs[:], scalar1=-1.0), ['cs'], ['cs'])
        V(_I('tensor_tensor', out=sn[:], in0=sn[:], in1=rho[:], op=ALU.mult), ['sn', 'rho'], ['sn'])
        V(_I('tensor_tensor', out=den[:], in0=lr[:], in1=lr[:], op=ALU.mult), ['lr'], ['den'])
        V(_I('tensor_tensor', out=tmpa[:], in0=li[:], in1=li[:], op=ALU.mult), ['li'], ['tmpa'])
        V(_I('tensor_tensor', out=den[:], in0=den[:], in1=tmpa[:], op=ALU.add), ['den', 'tmpa'], ['den'])
        V(_I('reciprocal', out=den[:], in_=den[:]), ['den'], ['den'])
        V(_I('tensor_tensor', out=kr[:], in0=cs[:], in1=lr[:], op=ALU.mult), ['cs', 'lr'], ['kr'])
        V(_I('tensor_tensor', out=tmpa[:], in0=sn[:], in1=li[:], op=ALU.mult), ['sn', 'li'], ['tmpa'])
        V(_I('tensor_tensor', out=kr[:], in0=kr[:], in1=tmpa[:], op=ALU.add), ['kr', 'tmpa'], ['kr'])
        V(_I('tensor_tensor', out=kr[:], in0=kr[:], in1=den[:], op=ALU.mult), ['kr', 'den'], ['kr'])
        V(_I('tensor_tensor', out=ki[:], in0=sn[:], in1=lr[:], op=ALU.mult), ['sn', 'lr'], ['ki'])
        V(_I('tensor_tensor', out=tmpa[:], in0=cs[:], in1=li[:], op=ALU.mult), ['cs', 'li'], ['tmpa'])
        V(_I('tensor_tensor', out=ki[:], in0=ki[:], in1=tmpa[:], op=ALU.subtract), ['ki', 'tmpa'], ['ki'])
        V(_I('tensor_tensor', out=ki[:], in0=ki[:], in1=den[:], op=ALU.mult), ['ki', 'den'], ['ki'])
        braw = [A.alloc("braw%d" % c, [128, 16, 16], F32) for c in range(2)]
        LDs(braw[0][:], ssm_b_re[l].rearrange("(q g) p h -> (g p) q h", g=2), [], ['braw0'])
        LDs(braw[1][:], ssm_b_im[l].rearrange("(q g) p h -> (g p) q h", g=2), [], ['braw1'])
        bb = [A.alloc("bb%d" % c, [128, 16, 16], F32) for c in range(2)]
        bt = A.alloc("bt", [128, 16, 16], F32)
        krb = kr[:, :].unsqueeze(2).to_broadcast([128, 16, 16])
        kib = ki[:, :].unsqueeze(2).to_broadcast([128, 16, 16])
        V(_I('tensor_tensor', out=bb[0][:], in0=braw[0][:], in1=krb, op=ALU.mult), ['braw0', 'kr'], ['bb0'])
        V(_I('tensor_tensor', out=bt[:], in0=braw[1][:], in1=kib, op=ALU.mult), ['braw1', 'ki'], ['bt'])
        V(_I('tensor_tensor', out=bb[0][:], in0=bb[0][:], in1=bt[:], op=ALU.subtract), ['bb0', 'bt'], ['bb0'])
        V(_I('tensor_tensor', out=bb[1][:], in0=braw[1][:], in1=krb, op=ALU.mult), ['braw1', 'kr'], ['bb1'])
        V(_I('tensor_tensor', out=bt[:], in0=braw[0][:], in1=kib, op=ALU.mult), ['braw0', 'ki'], ['bt'])
        V(_I('tensor_tensor', out=bb[1][:], in0=bb[1][:], in1=bt[:], op=ALU.add), ['bb1', 'bt'], ['bb1'])
        nat = A.alloc("nat", [128, 16, 128], F32)
        natv = nat[:].rearrange("p (qq r) (r2 g2 h) -> p qq r r2 g2 h", r=4, r2=4, g2=2)
        for c in range(2):
            V(_I('memset', nat[:], 0.0), [], ['nat'])
            bbv = bb[c][:].rearrange("p (qq r) h -> p qq r h", r=4)
            for r in range(4):
                for g in range(2):
                    V(_I('tensor_copy', out=natv[g * 64:(g + 1) * 64, :, r, r, g, :],
                                                                 in_=bbv[g * 64:(g + 1) * 64, :, r, :]),
                      ['bb%d' % c, 'nat'], ['nat'])
            for q in range(16):
                TR(psB[:, (q % 8) * 128:(q % 8 + 1) * 128], nat[:, q, :], ident_f[:], ['nat', 'ident_f'], ['pB%d' % ((q % 8) // 4)])
                if q % 8 == 7:
                    hq = q // 8
                    V(_I('tensor_copy', out=Bpad[c][:, hq * 8:(hq + 1) * 8, :],
                                                          in_=psB[:].rearrange("p (q m) -> p q m", q=8)),
                      ['pB0', 'pB1'], ['Bpad'])
        cnat = A.alloc("cnat", [128, 16, 128], F32)
        cnv = cnat[:].rearrange("p (ft r) (g s) -> p ft r g s", r=4, g=2)
        for c, (src, sgns) in enumerate(((ssm_c_re, ((0, 1.0), (1, -1.0))), (ssm_c_im, ((2, -1.0),)))):
            V(_I('memset', cnat[:], 0.0), [], ['cnat'])
            srcv = src[l].rearrange("(ft r g) h s -> r g h ft s", r=4, g=2)
            for r in range(4):
                for g in range(2):
                    p0 = 32 * r + 16 * g
                    LDs(cnv[p0:p0 + 16, :, r, g, :], srcv[r, g], ['cnat'], [('cnat', r, g)])
            ckeys = ['cnat'] + [('cnat', r, g) for r in range(4) for g in range(2)]
            for q in range(16):
                TR(psB[:, (q % 8) * 128:(q % 8 + 1) * 128], cnat[:, q, :], ident_f[:], ckeys + ['ident_f'], ['pB%d' % ((q % 8) // 4)])
                if q % 8 == 7:
                    hq = q // 8
                    for (ci, sg) in sgns:
                        Ac(_I('activation', out=Cpad[ci][:, hq * 8:(hq + 1) * 8, :],
                                                                       in_=psB[:].rearrange("p (q m) -> p q m", q=8),
                                                                       func=AF.Copy, scale=sg),
                           ['pB0', 'pB1'], ['Cpad'])
        dcol = A.alloc("dcol", [128, 4], F32)
        LDs(dcol[:], ssm_d[l].rearrange("(ft p) -> p ft", p=128), [], ['dcol'])
        for ft in range(4):
            V(_I('tensor_scalar_mul', out=Dd[:, ft, :], in0=ident_f[:], scalar1=dcol[:, ft:ft + 1]),
              ['dcol', 'ident_f'], ['Dd'])
        P.barrier()
        A.release(mk2)
        if 'uT' in taps and l == 0:
            tp = tap_out('uT', [4, 128, S], BF16)
            LD(tp.rearrange("k p s -> p k s"), uT[:], [], ['tap_uT'])
        if 'ssmw' in taps and l == 0:
            LD(tap_out('Bpad0', [128, 16, 128], BF16), Bpad[0][:], [], ['tap_b0'])
            LD(tap_out('Bpad1', [128, 16, 128], BF16), Bpad[1][:], [], ['tap_b1'])
            LD(tap_out('Cpad0', [128, 16, 128], BF16), Cpad[0][:], [], ['tap_c0'])
            LD(tap_out('Cpad2', [128, 16, 128], BF16), Cpad[2][:], [], ['tap_c2'])
            LD(tap_out('rho', [128, 16]), rho[:], [], ['tap_rho'])
            LD(tap_out('th', [128, 16]), th[:], [], ['tap_th'])
        if l == 0:
            stop_if('uT')
        HS = 1024
        wglu = A.alloc("wglu", [128, 4, 512], BF16)
        LD(wglu[:], w_glu[l].rearrange("(k p) f -> p k f", p=128), [], ['wglu'], q='pool')
        ygb = A.alloc("ygb", [128, 4, HS], BF16)
        carry = A.alloc("carry", [128, 16, 2], F32)
        V(_I('memset', carry[:], 0.0), [], ['carry'])
        nset = 2
        Ct = [A.alloc("Ct%d" % i, [128, HS], F32) for i in range(nset)]
        St = [A.alloc("St%d" % i, [128, HS], F32) for i in range(nset)]
        t1 = [A.alloc("t1%d" % i, [128, HS], F32) for i in range(nset)]
        t2 = [A.alloc("t2%d" % i, [128, HS], F32) for i in range(nset)]
        qre = [A.alloc("qre%d" % i, [128, HS], F32) for i in range(nset)]
        qim = [A.alloc("qim%d" % i, [128, HS], F32) for i in range(nset)]
        Pp = [A.alloc("Pp%d" % i, [128, 4, HS], BF16) for i in range(nset)]
        ph = A.alloc("ph", [128, HS], F32)
        pm = A.alloc("pm", [128, HS], F32)
        bur = [A.alloc("bur%d" % i, [128, HS], F32) for i in range(nset)]
        bui = [A.alloc("bui%d" % i, [128, HS], F32) for i in range(nset)]
        sgt = A.alloc("sgt", [128, 512], F32)
        YK = ['pB0', 'pB1']
        iters = [(half, ft, r) for half in range(2) for ft in range(4) for r in range(4)]

        def stage1a(i):
            half, ft, r = iters[i]
            q = 4 * ft + r
            s_ = i % nset
            t0 = half * HS
            k_ = lambda nm: (nm, s_)
            for c in range(2):
                for tc in range(2):
                    MM(psA[:, c * 1024 + tc * 512: c * 1024 + (tc + 1) * 512], Bpad[c][:, q, :],
                       uT[:, ft, t0 + tc * 512: t0 + (tc + 1) * 512], True, True,
                       ['Bpad', ('uT', ft)], ['pA%d' % (c * 2 + tc)])
            Ac(_I('activation', out=bur[s_][:], in_=psA[:, 0:1024], func=AF.Copy), ['pA0', 'pA1'], [k_('bur')])
            Ac(_I('activation', out=bui[s_][:], in_=psA[:, 1024:2048], func=AF.Copy), ['pA2', 'pA3'], [k_('bui')])

        def stage1b(i):
            half, ft, r = iters[i]
            q = 4 * ft + r
            s_ = i % nset
            t0 = half * HS
            k_ = lambda nm: (nm, s_)
            Ac(_I('activation', out=ph[:], in_=iota_t[:, t0:t0 + HS], func=AF.Identity,
                  bias=8.0 * math.pi, scale=th[:, q:q + 1]), ['iota_t', 'th'], ['ph'])
            range_reduce(ph[:], None, pm[:], ['ph'], ['ph', 'pm'])
            Ac(_I('activation', out=St[s_][:], in_=ph[:], func=AF.Sin), ['ph'], [k_('St')])
            Ac(_I('activation', out=pm[:], in_=ph[:], func=AF.Abs), ['ph'], ['pm'])
            Ac(_I('activation', out=Ct[s_][:], in_=pm[:], func=AF.Sin, bias=hpi_t[:, 0:1], scale=-1.0), ['pm'], [k_('Ct')])

        def stage2(i):
            half, ft, r = iters[i]
            q = 4 * ft + r
            s_ = i % nset
            k_ = lambda nm: (nm, s_)
            bure, buim = bur[s_][:], bui[s_][:]
            KRE, KIM = [k_('bur')], [k_('bui')]
            V(_I('tensor_tensor', out=t1[s_][:], in0=bure, in1=Ct[s_][:], op=ALU.mult), KRE + [k_('Ct')], [k_('t1')])
            V(_I('tensor_tensor', out=t2[s_][:], in0=buim, in1=St[s_][:], op=ALU.mult), KIM + [k_('St')], [k_('t2')])
            V(_I('tensor_tensor', out=qre[s_][:], in0=t1[s_][:], in1=t2[s_][:], op=ALU.add), [k_('t1'), k_('t2')], [k_('qre')])
            V(_I('tensor_tensor', out=t1[s_][:], in0=buim, in1=Ct[s_][:], op=ALU.mult), KIM + [k_('Ct')], [k_('t1')])
            V(_I('tensor_tensor', out=t2[s_][:], in0=bure, in1=St[s_][:], op=ALU.mult), KRE + [k_('St')], [k_('t2')])
            V(_I('tensor_tensor', out=qim[s_][:], in0=t1[s_][:], in1=t2[s_][:], op=ALU.subtract), [k_('t1'), k_('t2')], [k_('qim')])
            V(_I('tensor_tensor_scan', out=t1[s_][:], data0=rho[:, q:q + 1].to_broadcast([128, HS]), data1=qre[s_][:],
                 initial=carry[:, q, 0:1], op0=ALU.mult, op1=ALU.add), [k_('qre'), 'rho', ('carry', q)], [k_('t1')])
            V(_I('tensor_tensor_scan', out=t2[s_][:], data0=rho[:, q:q + 1].to_broadcast([128, HS]), data1=qim[s_][:],
                 initial=carry[:, q, 1:2], op0=ALU.mult, op1=ALU.add), [k_('qim'), 'rho', ('carry', q)], [k_('t2')])
            if half == 0:
                Ac(_I('activation', out=carry[:, q, 0:1], in_=t1[s_][:, HS - 1:HS], func=AF.Copy), [k_('t1')], [('carry', q)])
                Ac(_I('activation', out=carry[:, q, 1:2], in_=t2[s_][:, HS - 1:HS], func=AF.Copy), [k_('t2')], [('carry', q)])

        def stage3(i):
            half, ft, r = iters[i]
            q = 4 * ft + r
            s_ = i % nset
            t0 = half * HS
            k_ = lambda nm: (nm, s_)
            for pi_, (tt_, zz) in enumerate(((Ct, t1), (St, t2), (St, t1), (Ct, t2))):
                G(_I('tensor_tensor', out=Pp[s_][:, pi_, :], in0=tt_[s_][:], in1=zz[s_][:], op=ALU.mult),
                  [k_('Ct'), k_('St'), k_('t1'), k_('t2')], [(k_('Pp'), pi_)])
            for tc in range(2):
                for pi_, ci in enumerate((0, 1, 2, 2)):
                    MM(psB[:, tc * 512:(tc + 1) * 512], Cpad[ci][:, q, :], Pp[s_][:, pi_, tc * 512:(tc + 1) * 512],
                       (r == 0 and pi_ == 0), False, ['Cpad', (k_('Pp'), pi_)], ['pB%d' % tc])
            if r != 3:
                return
            for tc in range(2):
                MM(psB[:, tc * 512:(tc + 1) * 512], Dd[:, ft, :], uT[:, ft, t0 + tc * 512:t0 + (tc + 1) * 512], False, True,
                   ['Dd', ('uT', ft)], ['pB%d' % tc])
            Ac(_I('activation', out=ph[:], in_=psB[:], func=AF.Copy), YK, ['ph'])
            V(_I('tensor_tensor', out=pm[:], in0=ph[:], in1=ph[:], op=ALU.mult), ['ph'], ['pm'])
            V(_I('tensor_scalar', out=pm[:], in0=pm[:], scalar1=0.044715, scalar2=1.0, op0=ALU.mult, op1=ALU.add), ['pm'], ['pm'])
            V(_I('tensor_tensor', out=pm[:], in0=pm[:], in1=ph[:], op=ALU.mult), ['pm', 'ph'], ['pm'])
            Ac(_I('activation', out=pm[:], in_=pm[:], func=AF.Sigmoid, scale=1.5957691216057308), ['pm'], ['pm'])
            V(_I('tensor_tensor', out=ygb[:, ft, :], in0=pm[:], in1=ph[:], op=ALU.mult), ['pm', 'ph'], [('ygb', ft)])
            if ft != 3:
                return
            for fo in range(4):
                for tc in range(2):
                    for k in range(4):
                        MM(psB[:, tc * 512:(tc + 1) * 512], wglu[:, k, fo * 128:(fo + 1) * 128], ygb[:, k, tc * 512:(tc + 1) * 512],
                           k == 0, k == 3, ['wglu'] + [('ygb', f) for f in range(4)], ['pB%d' % tc])
                    Ac(_I('activation', out=sgt[:], in_=psB[:, tc * 512:(tc + 1) * 512], func=AF.Sigmoid), ['pB%d' % tc], ['sgt'])
                    V(_I('tensor_tensor', out=ys[0][:, fo, t0 + tc * 512:t0 + (tc + 1) * 512], in0=sgt[:],
                         in1=ygb[:, fo, tc * 512:(tc + 1) * 512], op=ALU.mult), ['sgt', ('ygb', fo)], [('ys', 0)])

        stage1a(0)
        stage1b(0)
        for i in range(len(iters)):
            if i + 1 < len(iters):
                stage1a(i + 1)
                stage1b(i + 1)
            stage2(i)
            stage3(i)
        P.barrier()
        A.release(mixer_mark)
        if 'ys0' in taps and l == 0:
            tp = tap_out('ys0', [4, 128, S], BF16)
            LD(tp.rearrange("k p s -> p k s"), ys[0][:], [], ['tap_ys0'])

        if l == 0:
            stop_if('ys0')
        mk = A.mark()
        wv = A.alloc("wv", [128, 8, 512], BF16)
        wg = A.alloc("wg", [128, 8, 512], BF16)
        load_win(wv, 1, 'wv')
        load_win(wg, 2, 'wg')
        cw = A.alloc("cw", [128, 4, 31], F32)
        cbias = A.alloc("cbias", [128, 4], F32)
        lng = A.alloc("lng", [128, 4], F32)
        lnb = A.alloc("lnb", [128, 4], F32)
        for ft in range(4):
            LDs(cw[:, ft, :], conf_dw_w[l][:, ft * 128:(ft + 1) * 128].rearrange("k p -> p k"), [], ['cw'])
        LDs(cbias[:], conf_dw_b[l].rearrange("(ft p) -> p ft", p=128), [], ['cbias'])
        LDs(lng[:], conf_ln_g[l].rearrange("(ft p) -> p ft", p=128), [], ['lng'])
        LDs(lnb[:], conf_ln_b[l].rearrange("(ft p) -> p ft", p=128), [], ['lnb'])
        zt = A.alloc("zt", [128, 32 + S], F32)
        cacc = A.alloc("cacc", [128, 4, S], F32)
        sgc = [A.alloc("sgc%d" % i, [128, 512], F32) for i in range(2)]
        cbt = [A.alloc("cbt%d" % i, [128, 512], BF16) for i in range(2)]
        cst = [A.alloc("cst%d" % i, [128, 512], BF16) for i in range(2)]
        mean_sb = A.alloc("mean_sb", [128, S], F32)
        rstd_sb = A.alloc("rstd_sb", [128, S], F32)
        V(_I('memset', zt[:, 0:32], 0.0), [], ['zt'])
        for ft in range(4):
            for tc in range(4):
                proj_in(wv, 'wv', ft, tc, psA[:, (tc % 2) * 512:(tc % 2 + 1) * 512], ('psA', 'v', tc % 2))
                proj_in(wg, 'wg', ft, tc, psA[:, 1024 + (tc % 2) * 512:1024 + (tc % 2 + 1) * 512], ('psA', 'g', tc % 2))
                Ac(_I('activation', out=sgc[tc % 2][:], in_=psA[:, 1024 + (tc % 2) * 512:1024 + (tc % 2 + 1) * 512],
                                                 func=AF.Sigmoid), [('psA', 'g', tc % 2)], [('sgc', tc % 2)])
                V(_I('tensor_tensor', out=zt[:, 32 + tc * 512:32 + (tc + 1) * 512],
                                                   in0=psA[:, (tc % 2) * 512:(tc % 2 + 1) * 512], in1=sgc[tc % 2][:], op=ALU.mult),
                  [('psA', 'v', tc % 2), ('sgc', tc % 2)], ['zt'])
            V(_I('tensor_scalar', out=cacc[:, ft, :], in0=zt[:, 32:32 + S], scalar1=cw[:, ft, 30:31],
                                               scalar2=cbias[:, ft:ft + 1], op0=ALU.mult, op1=ALU.add),
              ['zt', 'cw', 'cbias'], [('cacc', ft)])
            for k in range(30):
                V(_I('scalar_tensor_tensor', out=cacc[:, ft, :], in0=zt[:, 2 + k:2 + k + S], scalar=cw[:, ft, k:k + 1],
                                                               in1=cacc[:, ft, :], op0=ALU.mult, op1=ALU.add),
                  ['zt', 'cw'], [('cacc', ft)])
        for tc in range(4):
            for ft in range(4):
                i_ = (tc * 4 + ft) % 2
                Ac(_I('activation', out=cbt[i_][:], in_=cacc[:, ft, tc * 512:(tc + 1) * 512], func=AF.Copy),
                   [('cacc', ft)], [('cbt', i_)])
                Ac(_I('activation', out=cst[i_][:], in_=cacc[:, ft, tc * 512:(tc + 1) * 512], func=AF.Square),
                   [('cacc', ft)], [('cst', i_)])
                MM(psB[:, 0:512], ones_d[:], cbt[i_][:], ft == 0, ft == 3, ['ones_d', ('cbt', i_)], [('psB', 'mean')])
                MM(psB[:, 512:1024], ones_d[:], cst[i_][:], ft == 0, ft == 3, ['ones_d', ('cst', i_)], [('psB', 'msq')])
            sl = slice(tc * 512, (tc + 1) * 512)
            Ac(_I('activation', out=mean_sb[:, sl], in_=psB[:, 0:512], func=AF.Copy), [('psB', 'mean')], [('mean_sb', tc)])
            V(_I('tensor_tensor', out=rstd_sb[:, sl], in0=mean_sb[:, sl], in1=mean_sb[:, sl], op=ALU.mult),
              [('mean_sb', tc)], [('rstd_sb', tc)])
            V(_I('tensor_tensor', out=rstd_sb[:, sl], in0=psB[:, 512:1024], in1=rstd_sb[:, sl], op=ALU.subtract),
              [('psB', 'msq'), ('rstd_sb', tc)], [('rstd_sb', tc)])
            rstd_from_ss(rstd_sb[:, sl], rstd_sb[:, sl], 1.0, [('rstd_sb', tc)], [('rstd_sb', tc)])
        for ft in range(4):
            V(_I('tensor_tensor', out=cacc[:, ft, :], in0=cacc[:, ft, :], in1=mean_sb[:], op=ALU.subtract),
              [('cacc', ft)] + [('mean_sb', t) for t in range(4)], [('cacc', ft)])
            V(_I('tensor_tensor', out=cacc[:, ft, :], in0=cacc[:, ft, :], in1=rstd_sb[:], op=ALU.mult),
              [('cacc', ft)] + [('rstd_sb', t) for t in range(4)], [('cacc', ft)])
            Ac(_I('activation', out=ys[1][:, ft, :], in_=cacc[:, ft, :], func=AF.Silu, bias=lnb[:, ft:ft + 1],
                                             scale=lng[:, ft:ft + 1]), [('cacc', ft), 'lng', 'lnb'], [('ys', 1)])
        P.barrier()
        A.release(mk)
        if 'ys1' in taps and l == 0:
            tp = tap_out('ys1', [4, 128, S], BF16)
            LD(tp.rearrange("k p s -> p k s"), ys[1][:], [], ['tap_ys1'])

        if l == 0:
            stop_if('ys1')
        mk = A.mark()
        wsb = [A.alloc("wsb%d" % i, [128, 8, 512], BF16) for i in range(3)]
        for i in range(3):
            load_win(wsb[i], 3 + i, ('wsb', i))
        scw = A.alloc("scw", [128, 4, 3], F32)
        for ft in range(4):
            LDs(scw[:, ft, :], sconv_w[l][:, ft * 128:(ft + 1) * 128].rearrange("k p -> p k"), [], ['scw'])
        pt_ = A.alloc("pt_", [128, 2 + S], F32)
        bs = A.alloc("bs", [128, S], F32)
        cs_ = [A.alloc("cs_%d" % i, [128, 512], F32) for i in range(2)]
        sacc = A.alloc("sacc", [128, S], F32)
        V(_I('memset', pt_[:, 0:2], 0.0), [], ['pt_'])
        for ft in range(4):
            for tc in range(4):
                proj_in(wsb[0], ('wsb', 0), ft, tc, psA[:, 0:512], ('psA', 'b'))
                proj_in(wsb[1], ('wsb', 1), ft, tc, psA[:, 512:1024], ('psA', 'c'))
                proj_in(wsb[2], ('wsb', 2), ft, tc, psA[:, 1024:1536], ('psA', 'h'))
                Ac(_I('activation', out=bs[:, tc * 512:(tc + 1) * 512], in_=psA[:, 0:512], func=AF.Copy),
                   [('psA', 'b')], ['bs'])
                Ac(_I('activation', out=cs_[tc % 2][:], in_=psA[:, 512:1024], func=AF.Copy),
                   [('psA', 'c')], [('cs_', tc % 2)])
                V(_I('tensor_tensor', out=pt_[:, 2 + tc * 512:2 + (tc + 1) * 512], in0=psA[:, 1024:1536],
                                                   in1=cs_[tc % 2][:], op=ALU.mult), [('psA', 'h'), ('cs_', tc % 2)], ['pt_'])
            V(_I('tensor_scalar_mul', out=sacc[:], in0=pt_[:, 2:2 + S], scalar1=scw[:, ft, 2:3]), ['pt_', 'scw'], ['sacc'])
            for k in range(2):
                V(_I('scalar_tensor_tensor', out=sacc[:], in0=pt_[:, k:k + S], scalar=scw[:, ft, k:k + 1],
                                                               in1=sacc[:], op0=ALU.mult, op1=ALU.add), ['pt_', 'scw'], ['sacc'])
            V(_I('tensor_tensor', out=ys[2][:, ft, :], in0=sacc[:], in1=bs[:], op=ALU.mult), ['sacc', 'bs'], [('ys', 2)])
        P.barrier()
        A.release(mk)
        if 'ys2' in taps and l == 0:
            tp = tap_out('ys2', [4, 128, S], BF16)
            LD(tp.rearrange("k p s -> p k s"), ys[2][:], [], ['tap_ys2'])

        if l == 0:
            stop_if('ys2')
        mk = A.mark()
        mixT = A.alloc("mixT", [128, 8, S], BF16)
        wgt = [A.alloc("wgt%d" % n, [128, 8, 512], BF16) for n in range(3)]
        wbr = [A.alloc("wbr%d" % n, [128, 4, 512], BF16) for n in range(3)]
        bgc = A.alloc("bgc", [128, 3, 8], F32)
        for n in range(3):
            LDs(bgc[:, n, :], b_gate[l][n * D:(n + 1) * D].rearrange("(dc p) -> p dc", p=128), [], ['bgc'])
        gsb = [A.alloc("gsb%d" % n, [128, 512], F32) for n in range(3)]
        mt1 = A.alloc("mt1", [128, 512], F32)
        mt2 = A.alloc("mt2", [128, 512], F32)
        for dh in range(2):
            for n in range(3):
                LD(wgt[n][:], w_gate[l, :, n * D + dh * 512:n * D + (dh + 1) * 512].rearrange("(k p) f -> p k f", p=128),
                   [], [('wgt', n)], q='pool')
                LD(wbr[n][:], w_branch[l, n, :, dh * 512:(dh + 1) * 512].rearrange("(k p) f -> p k f", p=128),
                   [], [('wbr', n)], q='pool')
            for dcl in range(4):
                dc = dh * 4 + dcl
                for tc in range(4):
                    tsl = slice(tc * 512, (tc + 1) * 512)
                    for n in range(3):
                        gp = psA[:, n * 512:(n + 1) * 512]
                        for k in range(8):
                            MM(gp, wgt[n][:, k, dcl * 128:(dcl + 1) * 128], hT[:, k, tsl], k == 0, k == 7, [('wgt', n)], [('psA', 'g', n)])
                        bp = psA[:, 1536:2048] if n == 0 else psB[:, (n - 1) * 512:n * 512]
                        for k in range(4):
                            MM(bp, wbr[n][:, k, dcl * 128:(dcl + 1) * 128], ys[n][:, k, tsl], k == 0, k == 3, [('wbr', n)], [('ps', 'br', n)])
                        Ac(_I('activation', out=gsb[n][:], in_=gp, func=AF.Sigmoid, bias=bgc[:, n, dc:dc + 1]),
                           [('psA', 'g', n), 'bgc'], [('gsb', n)])
                    bps = [psA[:, 1536:2048], psB[:, 0:512], psB[:, 512:1024]]
                    V(_I('tensor_tensor', out=mt1[:], in0=bps[0], in1=gsb[0][:], op=ALU.mult),
                      [('ps', 'br', 0), ('gsb', 0)], ['mt1'])
                    V(_I('tensor_tensor', out=mt2[:], in0=bps[1], in1=gsb[1][:], op=ALU.mult),
                      [('ps', 'br', 1), ('gsb', 1)], ['mt2'])
                    V(_I('tensor_tensor', out=mt1[:], in0=mt1[:], in1=mt2[:], op=ALU.add), ['mt1', 'mt2'], ['mt1'])
                    V(_I('tensor_tensor', out=mt2[:], in0=bps[2], in1=gsb[2][:], op=ALU.mult),
                      [('ps', 'br', 2), ('gsb', 2)], ['mt2'])
                    V(_I('tensor_tensor', out=mixT[:, dc, tsl], in0=mt1[:], in1=mt2[:], op=ALU.add),
                      ['mt1', 'mt2'], [('mixT', dc)])
        P.barrier()
        mk = A.mark()
        if 'mixT' in taps and l == 0:
            tp = tap_out('mixT', [8, 128, S], BF16)
            LD(tp.rearrange("k p s -> p k s"), mixT[:], [], ['tap_mixT'])
        if l == 0:
            stop_if('mixT')
        rowG = load_row(2)
        wo = A.alloc("wo", [128, 8, D], BF16)
        LD(wo[:], w_out[l].rearrange("(k p) f -> p k f", p=128), [], ['wo'], q='pool')
        xt = [A.alloc("xt%d" % i, [128, D], F32) for i in range(2)]
        xo = [A.alloc("xo%d" % i, [128, D], F32) for i in range(2)]
        for t in range(NT):
            x_ = xt[t % 2]
            xo_ = xo[t % 2]
            LD(x_[:], xsrc[t * 128:(t + 1) * 128, :], [('xres', t)], [('xt', t % 2)])
            for hf in range(2):
                po = psA[:, ((t % 2) * 2 + hf) * 512:((t % 2) * 2 + hf + 1) * 512]
                for k in range(8):
                    MM(po, mixT[:, k, t * 128:(t + 1) * 128], wo[:, k, hf * 512:(hf + 1) * 512], k == 0, k == 7,
                       ['wo'], [('psA', 'o', t % 2, hf)])
                V(_I('tensor_tensor', out=xo_[:, hf * 512:(hf + 1) * 512], in0=po,
                                                                   in1=rowG[:, hf * 512:(hf + 1) * 512], op=ALU.mult),
                  [('psA', 'o', t % 2, hf), 'rowG'], [('xo', t % 2, hf)])
                V(_I('tensor_tensor', out=xo_[:, hf * 512:(hf + 1) * 512], in0=xo_[:, hf * 512:(hf + 1) * 512],
                                                                  in1=x_[:, hf * 512:(hf + 1) * 512], op=ALU.add),
                  [('xo', t % 2, hf), ('xt', t % 2)], [('xo', t % 2, hf)])
            LD(out[t * 128:(t + 1) * 128, :], xo_[:], [('xo', t % 2, 0), ('xo', t % 2, 1)], [('xres', t)])
        P.barrier()
        A.release(mk)
        A.release(base_mark)
        if 'xmix' in taps and l == 0:
            LD(tap_out('xmix', [S, D]), out, [], ['tap_xmix'], q='pool')
            P.barrier()

        if l == 0:
            stop_if('xmix')
        d_i = [A.alloc("d_i%d" % k, [128, NT], I32) for k in range(2)]
        wk = [A.alloc("wk%d" % k, [128, NT], F32) for k in range(2)]
        widx = A.alloc("widx", [128, NBLK], I32)
        widx2 = A.alloc("widx2", [128, NBLK, 4], I32)
        moe_mark = A.mark()
        h2tm = A.alloc("h2tm", [128, NT, D], BF16)
        lgr = A.alloc("lgr", [128, NT, 36], F32)
        mk = A.mark()
        rowA, rowB = load_rows(4, 3, norm_ffn_g)
        xt = [A.alloc("xt%d" % i, [128, D], F32) for i in range(2)]
        junk = A.alloc("junk", [128, D], BF16)
        htmp = A.alloc("htmp", [128, D], F32)
        h2f = [A.alloc("h2f%d" % i, [128, D], F32) for i in range(2)]
        h2T = [A.alloc("h2T%d" % i, [128, 8, 128], F32) for i in range(2)]
        ss = A.alloc("ss", [128, NT], F32)
        rstd = A.alloc("rstd", [128, NT], F32)
        wr = A.alloc("wr", [128, 8, 36], F32)
        LDs(wr[:, :, 0:4], w_rg[l].rearrange("(k p) g -> p k g", p=128), [], [('wr', 0)])
        LDs(wr[:, :, 4:36], w_re[l].rearrange("(k p) g -> p k g", p=128), [], [('wr', 1)])
        V(_I('memset', ss[:], 0.0), [], ['ss'])
        for t in range(NT):
            x_ = xt[t % 2]
            LD(x_[:], out[t * 128:(t + 1) * 128, :], [('xres', t)], [('xt', t % 2)])
            Ac(_I('activation', out=junk[:], in_=x_[:], func=AF.Square, accum_out=ss[:, t:t + 1]),
               [('xt', t % 2), 'ss'], ['junk', ('ss', t)])
            rstd_from_ss(ss[:, t:t + 1], rstd[:, t:t + 1], 1.0 / D, [('ss', t)], [('rstd', t)])
            V(_I('scalar_tensor_tensor', out=htmp[:], in0=x_[:], scalar=rstd[:, t:t + 1], in1=rowA[:],
                                                           op0=ALU.mult, op1=ALU.mult), [('xt', t % 2), ('rstd', t), 'rowA'], ['htmp'])
            hf_ = h2f[t % 2]
            V(_I('tensor_tensor', out=hf_[:], in0=htmp[:], in1=rowB[:], op=ALU.add),
              ['htmp', 'rowB'], [('h2f', t % 2)])
            Ac(_I('activation', out=h2tm[:, t, :], in_=hf_[:], func=AF.Copy), [('h2f', t % 2)], [('h2tm', t)])
            for k in range(8):
                TR(psB[:, k * 128:(k + 1) * 128], hf_[:, k * 128:(k + 1) * 128], ident_f[:],
                   [('h2f', t % 2), 'ident_f'], [('psB', 'tr')])
            hT_ = h2T[t % 2]
            V(_I('tensor_copy', out=hT_[:], in_=psB[:].rearrange("p (k m) -> p k m", k=8)),
              [('psB', 'tr')], [('h2T', t % 2)])
            pl = psA[:, (t % 2) * 512:(t % 2) * 512 + 36]
            for k in range(8):
                MM(pl, hT_[:, k, :], wr[:, k, :], k == 0, k == 7, [('h2T', t % 2), ('wr', 0), ('wr', 1)], [('psA', 'lg', t % 2)])
            Ac(_I('activation', out=lgr[:, t, :], in_=pl, func=AF.Copy), [('psA', 'lg', t % 2)], [('lgr', t)])
        P.barrier()
        A.release(mk)
        if 'lgr' in taps and l == 0:
            LD(tap_out('lgr', [128, NT, 36]), lgr[:], [], ['tap_lgr'])

        if l == 0:
            stop_if('lgr')
        mk = A.mark()
        rb = A.alloc("rb", [128, 36], F32)
        LD(rb[:, 0:4], b_rg[l].partition_broadcast(128), [], [('rb', 0)])
        LD(rb[:, 4:36], b_re[l].partition_broadcast(128), [], [('rb', 1)])
        lg = A.alloc("lg", [128, NT, 36], F32)
        R = ['r']
        V(_I('tensor_tensor', out=lg[:], in0=lgr[:], in1=rb[:, :].unsqueeze(1).to_broadcast([128, NT, 36]), op=ALU.add),
          [('rb', 0), ('rb', 1)], R)
        gmax = A.alloc("gmax", [128, NT], F32)
        gsh = A.alloc("gsh", [128, NT, 4], F32)
        gex = A.alloc("gex", [128, NT, 4], F32)
        gw = A.alloc("gw", [128, NT], F32)
        pen = A.alloc("pen", [128, NT, 4], F32)
        em = A.alloc("em", [128, NT, 32], F32)
        em2 = A.alloc("em2", [128, NT, 32], F32)
        m1 = A.alloc("m1", [128, NT], F32)
        m2 = A.alloc("m2", [128, NT], F32)
        oh = [A.alloc("oh%d" % k, [128, NT, 32], F32) for k in range(2)]
        ohb = A.alloc("ohb", [128, NT * 32], BF16)
        V(_I('tensor_reduce', out=gmax[:], in_=lg[:, :, 0:4], axis=AX.X, op=ALU.max), R, R)
        V(_I('tensor_tensor', out=gsh[:], in0=lg[:, :, 0:4], in1=gmax[:, :].unsqueeze(2).to_broadcast([128, NT, 4]),
                                    op=ALU.subtract), R, R)
        Ac(_I('activation', out=gex[:], in_=gsh[:], func=AF.Exp), R, R)
        V(_I('tensor_reduce', out=gw[:], in_=gex[:], axis=AX.X, op=ALU.add), R, R)
        V(_I('reciprocal', out=gw[:], in_=gw[:]), R, R)
        V(_I('tensor_single_scalar', out=pen[:], in_=gsh[:], scalar=0.0, op=ALU.is_equal), R, R)
        V(_I('tensor_scalar', out=pen[:], in0=pen[:], scalar1=-1.0, scalar2=1e30, op0=ALU.add, op1=ALU.mult), R, R)
        V(_I('tensor_tensor', out=em[:].rearrange("p t (g x) -> p t g x", x=8),
                                    in0=lg[:, :, 4:36].rearrange("p t (g x) -> p t g x", x=8),
                                    in1=pen[:, :, :].unsqueeze(3).to_broadcast([128, NT, 4, 8]), op=ALU.add), R, R)
        V(_I('tensor_reduce', out=m1[:], in_=em[:], axis=AX.X, op=ALU.max), R, R)
        V(_I('tensor_tensor', out=oh[0][:], in0=em[:], in1=m1[:, :].unsqueeze(2).to_broadcast([128, NT, 32]),
                                    op=ALU.is_equal), R, R)
        V(_I('scalar_tensor_tensor', out=em2[:], in0=oh[0][:], scalar=-1e30, in1=em[:], op0=ALU.mult, op1=ALU.add), R, R)
        V(_I('tensor_reduce', out=m2[:], in_=em2[:], axis=AX.X, op=ALU.max), R, R)
        V(_I('tensor_tensor', out=oh[1][:], in0=em2[:], in1=m2[:, :].unsqueeze(2).to_broadcast([128, NT, 32]),
                                    op=ALU.is_equal), R, R)
        V(_I('tensor_tensor', out=m2[:], in0=m2[:], in1=m1[:], op=ALU.subtract), R, R)
        Ac(_I('activation', out=m2[:], in_=m2[:], func=AF.Exp), R, R)
        V(_I('tensor_scalar_add', out=m2[:], in0=m2[:], scalar1=1.0), R, R)
        V(_I('reciprocal', out=m2[:], in_=m2[:]), R, R)
        V(_I('tensor_tensor', out=wk[0][:], in0=m2[:], in1=gw[:], op=ALU.mult), R, R)
        V(_I('tensor_tensor', out=wk[1][:], in0=gw[:], in1=wk[0][:], op=ALU.subtract), R, R)
        V(_I('tensor_tensor', out=ohb[:], in0=oh[0][:].rearrange("p t x -> p (t x)"),
                                    in1=oh[1][:].rearrange("p t x -> p (t x)"), op=ALU.add), R, R)
        MM(psA[:, 0:512], ustrict[:], ohb[:], True, True, R + ['ustrict'], [('psA', 'pre')])
        MM(psA[:, 512:1024], ones1[:], ohb[:], True, True, R + ['ones1'], [('psA', 'tot')])
        scan_mask = A.alloc("scan_mask", [128, NE, NT], F32)
        V(_I('memset', scan_mask[:], 1.0), [], ['scan_mask'])
        V(_I('memset', scan_mask[:, :, 0:1], 0.0), ['scan_mask'], ['scan_mask'])
        tot = A.alloc("tot", [128, NE, NT], F32)
        cumi = A.alloc("cumi", [128, NE, NT], F32)
        Ac(_I('activation', out=tot[:], in_=psA[:, 512:1024].rearrange("p (t x) -> p x t", x=32), func=AF.Copy),
           [('psA', 'tot')], R)
        V(_I('tensor_tensor_scan', out=cumi[:].rearrange("p x t -> p (x t)"), data0=scan_mask[:].rearrange("p x t -> p (x t)"),
                                         data1=tot[:].rearrange("p x t -> p (x t)"), initial=0.0, op0=ALU.mult, op1=ALU.add),
          R + ['scan_mask'], R)
        npad = A.alloc("npad", [128, NE], F32)
        ntmp = A.alloc("ntmp", [128, NE], F32)
        pend = A.alloc("pend", [128, NE], F32)
        cmpn = A.alloc("cmpn", [128, NE, NT], F32)
        V(_I('tensor_tensor', out=cmpn[:], in0=cumi[:, :, NT - 1:NT].to_broadcast([128, NE, NT]),
                                    in1=b128[:, 0:NT].unsqueeze(1).to_broadcast([128, NE, NT]), op=ALU.is_gt), R + ['b128'], R)
        V(_I('tensor_reduce', out=npad[:], in_=cmpn[:], axis=AX.X, op=ALU.add), R, R)
        V(_I('tensor_scalar_mul', out=npad[:], in0=npad[:], scalar1=128.0), R, R)
        V(_I('tensor_tensor_scan', out=pend[:], data0=ones_row[:], data1=npad[:], initial=0.0, op0=ALU.mult, op1=ALU.add),
          R + ['ones_row'], R)
        V(_I('tensor_tensor', out=cumi[:], in0=cumi[:], in1=tot[:], op=ALU.subtract), R, R)
        V(_I('tensor_tensor', out=ntmp[:], in0=pend[:], in1=npad[:], op=ALU.subtract), R, R)
        V(_I('tensor_tensor', out=cumi[:], in0=cumi[:], in1=ntmp[:, :].unsqueeze(2).to_broadcast([128, NE, NT]), op=ALU.add), R, R)
        dest = A.alloc("dest", [128, NT, NE], F32)
        V(_I('tensor_tensor', out=dest[:], in0=psA[:, 0:512].rearrange("p (t x) -> p t x", x=32),
                                    in1=cumi[:].rearrange("p x t -> p t x"), op=ALU.add), R + [('psA', 'pre')], R)
        dsel = A.alloc("dsel", [128, NT, NE], F32)
        dfl = A.alloc("dfl", [128, NT], F32)
        for k in range(2):
            V(_I('tensor_tensor', out=dsel[:], in0=dest[:], in1=oh[k][:], op=ALU.mult), R, R)
            V(_I('tensor_reduce', out=dfl[:], in_=dsel[:], axis=AX.X, op=ALU.add), R, R)
            V(_I('tensor_copy', out=d_i[k][:], in_=dfl[:]), R, [('d_i', k)])
        cmp_ = A.alloc("cmp_", [128, NBLK, NE], F32)
        bef = A.alloc("bef", [128, NBLK], F32)
        V(_I('tensor_tensor', out=cmp_[:], in0=pend[:, :].unsqueeze(1).to_broadcast([128, NBLK, NE]),
                                    in1=b128[:, :].unsqueeze(2).to_broadcast([128, NBLK, NE]), op=ALU.is_le), R + ['b128'], R)
        V(_I('tensor_reduce', out=bef[:], in_=cmp_[:], axis=AX.X, op=ALU.add), R, R)
        V(_I('tensor_scalar_min', out=bef[:], in0=bef[:], scalar1=float(NE - 1)), R, R)
        basep = A.alloc("basep", [128, 1], F32)
        wif = A.alloc("wif", [128, NBLK], F32)
        G(_I('iota', basep[:], pattern=[[0, 1]], base=l * NE * 128, channel_multiplier=1,
             allow_small_or_imprecise_dtypes=True), [], ['basep'])
        V(_I('tensor_scalar', out=wif[:], in0=bef[:], scalar1=128.0, scalar2=basep[:, 0:1], op0=ALU.mult, op1=ALU.add),
          R + ['basep'], R)
        V(_I('tensor_copy', out=widx[:], in_=wif[:]), R, ['widx'])
        base2 = A.alloc("base2", [128, 4], F32)
        wif2 = A.alloc("wif2", [128, NBLK, 4], F32)
        G(_I('iota', base2[:], pattern=[[128, 4]], base=l * NE * W, channel_multiplier=1,
             allow_small_or_imprecise_dtypes=True), [], ['base2'])
        V(_I('scalar_tensor_tensor', out=wif2[:], in0=bef[:, :].unsqueeze(2).to_broadcast([128, NBLK, 4]), scalar=float(W),
             in1=base2[:, :].unsqueeze(1).to_broadcast([128, NBLK, 4]), op0=ALU.mult, op1=ALU.add), R + ['base2'], R)
        V(_I('tensor_copy', out=widx2[:], in_=wif2[:]), R, ['widx2'])
        if 'route' in taps and l == 0:
            LD(tap_out('d_i0', [128, NT], I32), d_i[0][:], [('d_i', 0)], ['tap_d0'])
            LD(tap_out('d_i1', [128, NT], I32), d_i[1][:], [('d_i', 1)], ['tap_d1'])
            LD(tap_out('wk0', [128, NT]), wk[0][:], R, ['tap_w0'])
            LD(tap_out('wk1', [128, NT]), wk[1][:], R, ['tap_w1'])
            LD(tap_out('widx', [128, NBLK], I32), widx[:], ['widx'], ['tap_be'])
        if l == 0:
            stop_if('route')
        P.barrier()
        A.release(mk)

        for t in range(NT):
            for k in range(2):
                P.dma('pool', _I('indirect_dma_start',
                    out=xb_d, out_offset=bass.IndirectOffsetOnAxis(ap=d_i[k][:, t:t + 1], axis=0),
                    in_=h2tm[:, t, :], in_offset=None), [], [('xb_d', t, k)])
        P.barrier()

        A.release(moe_mark)
        mk = A.mark()
        w1s = [A.alloc("w1s%d" % i, [128, 8, W], F32) for i in range(2)]
        w3s = [A.alloc("w3s%d" % i, [128, 8, W], F32) for i in range(2)]
        w2s = [A.alloc("w2s%d" % i, [128, 4, D], F32) for i in range(2)]
        w1b = [A.alloc("w1b%d" % i, [128, 8, W], BF16) for i in range(2)]
        w3b = [A.alloc("w3b%d" % i, [128, 8, W], BF16) for i in range(2)]
        w2b = [A.alloc("w2b%d" % i, [128, 4, D], BF16) for i in range(2)]
        xbt = [A.alloc("xbt%d" % i, [128, D], BF16) for i in range(2)]
        xbT = [A.alloc("xbT%d" % i, [128, 8, 128], BF16) for i in range(2)]
        sgh = [A.alloc("sgh%d" % i, [128, 512], F32) for i in range(2)]
        hidT = [A.alloc("hidT%d" % i, [128, 4, 128], BF16) for i in range(2)]
        ybs = [A.alloc("ybs%d" % i, [128, D], F32) for i in range(2)]
        eg_rows = w_eg.rearrange("l e (p j) f -> (l e p) (j f)", j=8)
        eu_rows = w_eu.rearrange("l e (p j) f -> (l e p) (j f)", j=8)
        ed_rows = w_ed.rearrange("l e r f -> (l e r) f")

        def issue_loads(b):
            wi = b % 2
            for (dst, rows_, key) in ((w1s, eg_rows, 'w1s'), (w3s, eu_rows, 'w3s')):
                P.dma('pool', _I('indirect_dma_start', out=dst[wi][:].rearrange("p k f -> p (k f)"), out_offset=None, in_=rows_,
                                 in_offset=bass.IndirectOffsetOnAxis(ap=widx[:, b:b + 1], axis=0)), [], [(key, wi)])
            for k in range(4):
                P.dma('pool', _I('indirect_dma_start', out=w2s[wi][:, k, :], out_offset=None, in_=ed_rows,
                                 in_offset=bass.IndirectOffsetOnAxis(ap=widx2[:, b, k:k + 1], axis=0)), [], [('w2s', wi, k)])
            LD(xbt[wi][:], xb_d[b * 128:(b + 1) * 128, :], [], [('xbt', wi)])
        issue_loads(0)
        for b in range(NBLK):
            i_ = b % 2
            wi = b % 2
            if b + 1 < NBLK:
                issue_loads(b + 1)
            V(_I('tensor_copy', out=w1b[wi][:], in_=w1s[wi][:]), [('w1s', wi)], [('w1b', wi)])
            V(_I('tensor_copy', out=w3b[wi][:], in_=w3s[wi][:]), [('w3s', wi)], [('w3b', wi)])
            V(_I('tensor_copy', out=w2b[wi][:], in_=w2s[wi][:]), [('w2s', wi, k) for k in range(4)], [('w2b', wi)])
            xv = xbt[i_][:].rearrange("s (p j) -> s j p", j=8)
            for k in range(8):
                TR(psT[:, i_, k * 128:(k + 1) * 128], xv[:, k, :], ident_b[:], [('xbt', i_)], [('psT', i_)])
            V(_I('tensor_copy', out=xbT[i_][:], in_=psT[:, i_, :].rearrange("p (k m) -> p k m", k=8)),
              [('psT', i_)], [('xbT', i_)])
            for (wt, wkey, off) in ((w1b[wi], ('w1b', wi), 0), (w3b[wi], ('w3b', wi), 512)):
                for fc in range(4):
                    for k in range(8):
                        MM(psA[:, i_ * 1024 + off + fc * 128: i_ * 1024 + off + (fc + 1) * 128], wt[:, k, fc * 128:(fc + 1) * 128],
                           xbT[i_][:, k, :], k == 0, k == 7, [wkey, ('xbT', i_)], [('psA', 'gu', i_, off)])
            Ac(_I('activation', out=sgh[i_][:], in_=psA[:, i_ * 1024:i_ * 1024 + 512], func=AF.Silu),
               [('psA', 'gu', i_, 0)], [('sgh', i_)])
            V(_I('tensor_tensor', out=hidT[i_][:].rearrange("p f m -> p (f m)"), in0=psA[:, i_ * 1024 + 512:i_ * 1024 + 1024],
                                               in1=sgh[i_][:], op=ALU.mult), [('psA', 'gu', i_, 512), ('sgh', i_)], [('hidT', i_)])
            for hf in range(2):
                for fc in range(4):
                    MM(psB[:, hf * 512:(hf + 1) * 512], hidT[i_][:, fc, :], w2b[wi][:, fc, hf * 512:(hf + 1) * 512], fc == 0, fc == 3,
                       [('hidT', i_), ('w2b', wi)], [('psB', 'yb')])
            Ac(_I('activation', out=ybs[i_][:], in_=psB[:], func=AF.Copy), [('psB', 'yb')], [('ybs', i_)])
            LD(yb_d[b * 128:(b + 1) * 128, :], ybs[i_][:], [('ybs', i_)], [('yb_d', b)])
        P.barrier()
        A.release(mk)

        mk = A.mark()
        rowG = load_row(5)
        g1t = [A.alloc("g1t%d" % i, [128, D], F32) for i in range(2)]
        g2t = [A.alloc("g2t%d" % i, [128, D], F32) for i in range(2)]
        xt = [A.alloc("xt%d" % i, [128, D], F32) for i in range(2)]
        last = (l == n_layers - 1) and do_final
        if last:
            fg = A.alloc("fg", [128, D], F32)
            LD(fg[:], fin_g.partition_broadcast(128), [], ['fg'])
            ss = A.alloc("ss", [128, NT], F32)
            rstd = A.alloc("rstd", [128, NT], F32)
            junk = A.alloc("junk", [128, D], BF16)
            V(_I('memset', ss[:], 0.0), [], ['ss'])
        for t in range(NT):
            i_ = t % 2
            P.dma('pool', _I('indirect_dma_start',
                out=g1t[i_][:], out_offset=None, in_=yb_d,
                in_offset=bass.IndirectOffsetOnAxis(ap=d_i[0][:, t:t + 1], axis=0)), [], [('g1t', i_)])
            P.dma('pool', _I('indirect_dma_start',
                out=g2t[i_][:], out_offset=None, in_=yb_d,
                in_offset=bass.IndirectOffsetOnAxis(ap=d_i[1][:, t:t + 1], axis=0)), [], [('g2t', i_)])
            LD(xt[i_][:], out[t * 128:(t + 1) * 128, :], [('xres', t)], [('xt', i_)])
            V(_I('tensor_scalar_mul', out=g1t[i_][:], in0=g1t[i_][:], scalar1=wk[0][:, t:t + 1]), [('g1t', i_)], [('g1t', i_)])
            V(_I('scalar_tensor_tensor', out=g1t[i_][:], in0=g2t[i_][:], scalar=wk[1][:, t:t + 1], in1=g1t[i_][:],
                                                           op0=ALU.mult, op1=ALU.add), [('g1t', i_), ('g2t', i_)], [('g1t', i_)])
            V(_I('tensor_tensor', out=g1t[i_][:], in0=g1t[i_][:], in1=rowG[:], op=ALU.mult), [('g1t', i_), 'rowG'], [('g1t', i_)])
            V(_I('tensor_tensor', out=xt[i_][:], in0=xt[i_][:], in1=g1t[i_][:], op=ALU.add), [('g1t', i_), ('xt', i_)], [('xt', i_)])
            if last:
                Ac(_I('activation', out=junk[:], in_=xt[i_][:], func=AF.Square, accum_out=ss[:, t:t + 1]),
                   [('xt', i_), 'ss'], ['junk', ('ss', t)])
                rstd_from_ss(ss[:, t:t + 1], rstd[:, t:t + 1], 1.0 / D, [('ss', t)], [('rstd', t)])
                V(_I('scalar_tensor_tensor', out=xt[i_][:], in0=xt[i_][:], scalar=rstd[:, t:t + 1], in1=fg[:],
                                                               op0=ALU.mult, op1=ALU.mult), [('xt', i_), ('rstd', t), 'fg'], [('xt', i_)])
            LD(out[t * 128:(t + 1) * 128, :], xt[i_][:], [('xt', i_)], [('xres', t)])
        P.barrier()
        A.release(mk)

    P.barrier()
    P.emit(st)
    st.close()
    print("SBUF peak bytes", A.peak, "limit", A.limit)
    return nc, list(tap_aps.keys())


INPUT_NAMES = ['x', 'c', 'norm_mix_g', 'norm_ffn_g', 'w_ada', 'b_ada', 'w_in', 'lam_re', 'lam_im', 'log_step',
               'ssm_b_re', 'ssm_b_im', 'ssm_c_re', 'ssm_c_im', 'ssm_d', 'w_glu', 'conf_dw_w', 'conf_dw_b',
               'conf_ln_g', 'conf_ln_b', 'sconv_w', 'w_branch', 'w_gate', 'b_gate', 'w_out', 'w_router_group',
               'b_router_group', 'w_router_expert', 'b_router_expert', 'w_exp_gate', 'w_exp_up', 'w_exp_down',
               'final_norm_g']

_CACHE = {}


def kernel(**inputs):
    if 'nc' not in _CACHE:
        _CACHE['nc'] = build_program()[0]
    nc = _CACHE['nc']
    arrs = {k: np.ascontiguousarray(np.asarray(inputs[k], dtype=np.float32)) for k in INPUT_NAMES}
    in_maps = []
    for b in range(8):
        m = {k: v for k, v in arrs.items() if k not in ('x', 'c')}
        m['x'] = np.ascontiguousarray(arrs['x'][b])
        m['c'] = np.ascontiguousarray(arrs['c'][b])
        in_maps.append(m)
    res = run_bass_kernel_spmd(nc, in_maps, core_ids=list(range(8)))
    return np.stack([np.asarray(r["out"], dtype=np.float32) for r in res.results], axis=0)
```
